# Optimizing a Trainium2 kernel written in Bass

```python
import math
import jax, jax.numpy as jnp
from jax import lax
import numpy as np

D_MODEL = 1024
BATCH = 8
SEQ = 4096
DEPTH = 2

ATTN_HEADS = 8
ATTN_HEAD_DIM = 64
ATTN_WIDTH = ATTN_HEADS * ATTN_HEAD_DIM
KV_RANK = 256
IDX_HEADS = 8
IDX_DIM = 64
TOPK_MAX = 256
ATTN_QBLOCK = 64
N_BUCKETS = 32
MAX_DISTANCE = 128
SSM_HEADS = 16
SSM_HEAD_DIM = 64
SSM_INNER = SSM_HEADS * SSM_HEAD_DIM
SSM_GROUPS = 2
SSM_STATE = 128
CONV_WIDTH = 4
CONV_CH = SSM_INNER + 2 * SSM_GROUPS * SSM_STATE
SSD_CHUNK = 128
N_EXPERTS = 32
TOP_K = 4
D_EXPERT = D_MODEL
SWIGLU_LIMIT = 7.0
SWIGLU_ALPHA = 1.702
MOE_BLOCK = 128
EPS = 1e-6

SPLIT_SIZES = (ATTN_WIDTH, KV_RANK, IDX_HEADS * IDX_DIM, IDX_DIM, IDX_HEADS, SSM_INNER, CONV_CH, SSM_HEADS, 2 * D_MODEL)
D_IN = sum(SPLIT_SIZES)

kernel_name = 'hybrid_dsa_ssd_moe_adaln'


def _rms(x, g, eps=EPS):
    xf = x.astype(jnp.float32)
    y = xf * lax.rsqrt(jnp.mean(xf * xf, axis=-1, keepdims=True) + eps)
    return (y * g.astype(jnp.float32)).astype(x.dtype)


def _layernorm(x, g, b, eps=EPS):
    xf = x.astype(jnp.float32)
    mu = jnp.mean(xf, axis=-1, keepdims=True)
    var = jnp.mean(jnp.square(xf - mu), axis=-1, keepdims=True)
    y = (xf - mu) * lax.rsqrt(var + eps) * g.astype(jnp.float32) + b.astype(jnp.float32)
    return y.astype(x.dtype)


def _t5_bucket(dist):
    n = jnp.maximum(dist, 0)
    max_exact = N_BUCKETS // 2
    nf = jnp.maximum(n, 1).astype(jnp.float32)
    large = max_exact + (jnp.log(nf / max_exact) / math.log(MAX_DISTANCE / max_exact) * (N_BUCKETS - max_exact)).astype(jnp.int32)
    large = jnp.minimum(large, N_BUCKETS - 1)
    return jnp.where(n < max_exact, n, large)


def _split(proj):
    idx = [int(i) for i in np.cumsum(SPLIT_SIZES)[:-1]]
    return jnp.split(proj, idx, axis=-1)


def _dsa_attention(q, k, v, q_idx, w_idx, k_idx, rel_bias):
    Bsz, L = q.shape[0], q.shape[1]
    topk = min(TOPK_MAX, L // 4)
    nblk = L // ATTN_QBLOCK
    scale = ATTN_HEAD_DIM ** -0.5
    key_pos = jnp.arange(L, dtype=jnp.int32)

    def to_blocks(a):
        return jnp.moveaxis(a.reshape(Bsz, nblk, ATTN_QBLOCK, *a.shape[2:]), 1, 0)

    def block(args):
        qb, qib, wib, start = args
        t = start + jnp.arange(ATTN_QBLOCK, dtype=jnp.int32)
        dots = jnp.einsum('bqhd,bsd->bqhs', qib, k_idx)
        score = jnp.einsum('bqh,bqhs->bqs', wib, jax.nn.relu(dots)).astype(jnp.float32)
        causal = key_pos[None, :] <= t[:, None]
        score = jnp.where(causal[None], score, -jnp.inf)
        _, sel = lax.top_k(score, topk)
        kg = jax.vmap(lambda kk, ii: kk[ii])(k, sel)
        vg = jax.vmap(lambda vv, ii: vv[ii])(v, sel)
        dist = t[None, :, None] - sel
        bias = jnp.moveaxis(rel_bias[_t5_bucket(dist)], -1, 2).astype(jnp.float32)
        logits = jnp.einsum('bqhd,bqkhd->bqhk', qb, kg).astype(jnp.float32) * scale + bias
        logits = jnp.where((dist >= 0)[:, :, None, :], logits, -jnp.inf)
        p = jax.nn.softmax(logits, axis=-1).astype(vg.dtype)
        return jnp.einsum('bqhk,bqkhd->bqhd', p, vg)

    starts = jnp.arange(nblk, dtype=jnp.int32) * ATTN_QBLOCK
    out = lax.map(block, (to_blocks(q), to_blocks(q_idx), to_blocks(w_idx), starts))
    return jnp.moveaxis(out, 0, 1).reshape(Bsz, L, ATTN_WIDTH)


def _mamba2_ssd(z, xbc, dt_raw, conv_w, conv_b, dt_bias, a_log, d_skip, norm_w):
    Bsz, L = xbc.shape[0], xbc.shape[1]
    nc = L // SSD_CHUNK
    hg = SSM_HEADS // SSM_GROUPS
    xbc = lax.conv_general_dilated(xbc, conv_w[:, None, :].astype(xbc.dtype), window_strides=(1,),
                                   padding=[(CONV_WIDTH - 1, 0)], dimension_numbers=('NWC', 'WIO', 'NWC'),
                                   feature_group_count=CONV_CH) + conv_b
    xbc = jax.nn.silu(xbc).astype(jnp.float32)
    xs, bm, cm = jnp.split(xbc, [SSM_INNER, SSM_INNER + SSM_GROUPS * SSM_STATE], axis=-1)
    dt = jax.nn.softplus(dt_raw.astype(jnp.float32) + dt_bias.astype(jnp.float32))
    a = -jnp.exp(a_log.astype(jnp.float32)).reshape(SSM_GROUPS, hg)

    def chunks(arr, *tail):
        return jnp.moveaxis(arr.reshape(Bsz, nc, SSD_CHUNK, *tail), 1, 0)

    x_c = chunks(xs, SSM_GROUPS, hg, SSM_HEAD_DIM)
    dt_c = chunks(dt, SSM_GROUPS, hg)
    b_c = chunks(bm, SSM_GROUPS, SSM_STATE)
    c_c = chunks(cm, SSM_GROUPS, SSM_STATE)
    causal = jnp.tril(jnp.ones((SSD_CHUNK, SSD_CHUNK), dtype=bool))[None, :, :, None, None]

    def step(state, inp):
        xc, dtc, bc, cc = inp
        acum = jnp.cumsum(dtc * a, axis=1)
        seg = acum[:, :, None] - acum[:, None, :]
        lmat = jnp.exp(jnp.where(causal, seg, -jnp.inf))
        cb = jnp.einsum('bign,bjgn->bijg', cc, bc)
        wmat = cb[..., None] * lmat * dtc[:, None]
        y = jnp.einsum('bijgh,bjghp->bighp', wmat, xc)
        y = y + jnp.einsum('bign,bghpn->bighp', cc, state) * jnp.exp(acum)[..., None]
        decay = jnp.exp(acum[:, -1:] - acum) * dtc
        state = state * jnp.exp(acum[:, -1])[..., None, None] + jnp.einsum('bjgn,bjgh,bjghp->bghpn', bc, decay, xc)
        return state, y

    h0 = jnp.zeros((Bsz, SSM_GROUPS, hg, SSM_HEAD_DIM, SSM_STATE), jnp.float32)
    _, ys = lax.scan(step, h0, (x_c, dt_c, b_c, c_c))
    y = jnp.moveaxis(ys, 0, 1).reshape(Bsz, L, SSM_HEADS, SSM_HEAD_DIM)
    y = y + d_skip.astype(jnp.float32)[:, None] * xs.reshape(Bsz, L, SSM_HEADS, SSM_HEAD_DIM)
    y = y.reshape(Bsz, L, SSM_INNER) * jax.nn.silu(z.astype(jnp.float32))
    yg = y.reshape(Bsz, L, SSM_GROUPS, SSM_INNER // SSM_GROUPS)
    yg = yg * lax.rsqrt(jnp.mean(yg * yg, axis=-1, keepdims=True) + EPS)
    y = yg.reshape(Bsz, L, SSM_INNER) * norm_w.astype(jnp.float32)
    return y.astype(z.dtype)


def _moe(h, w_router, b_router, w_gu, b_gu, w_dn, b_dn):
    Bsz, L, D = h.shape
    T = Bsz * L
    hf = h.reshape(T, D)
    logits = (hf @ w_router + b_router).astype(jnp.float32)
    top_val, top_idx = lax.top_k(logits, TOP_K)
    gates = jax.nn.softmax(top_val, axis=-1)
    n_assign = T * TOP_K
    e_flat = top_idx.reshape(n_assign)
    g_flat = gates.reshape(n_assign)
    tok_flat = jnp.arange(n_assign, dtype=jnp.int32) // TOP_K
    order = jnp.argsort(e_flat)
    e_sorted = e_flat[order]
    counts = jnp.bincount(e_flat, length=N_EXPERTS)
    padded = (counts + MOE_BLOCK - 1) // MOE_BLOCK * MOE_BLOCK
    pad_end = jnp.cumsum(padded)
    pad_start = pad_end - padded
    start = jnp.cumsum(counts) - counts
    rank = jnp.arange(n_assign, dtype=jnp.int32) - start[e_sorted]
    dest = pad_start[e_sorted] + rank
    n_rows = n_assign + N_EXPERTS * MOE_BLOCK
    row_tok = jnp.full((n_rows,), T, dtype=jnp.int32).at[dest].set(tok_flat[order])
    row_gate = jnp.zeros((n_rows,), jnp.float32).at[dest].set(g_flat[order])
    n_blk = n_rows // MOE_BLOCK
    blk_start = jnp.arange(n_blk, dtype=jnp.int32) * MOE_BLOCK
    blk_exp = jnp.minimum(jnp.sum(blk_start[:, None] >= pad_end[None, :], axis=1), N_EXPERTS - 1)
    h_pad = jnp.concatenate([hf, jnp.zeros((1, D), hf.dtype)], axis=0)

    def run_block(args):
        toks, e = args
        xb = h_pad[toks]
        gu = xb @ w_gu[e] + b_gu[e]
        g, u = gu[:, :D_EXPERT], gu[:, D_EXPERT:]
        g = jnp.minimum(g, SWIGLU_LIMIT)
        u = jnp.clip(u, -SWIGLU_LIMIT, SWIGLU_LIMIT)
        act = (u + 1.0) * (g * jax.nn.sigmoid(SWIGLU_ALPHA * g))
        return act @ w_dn[e] + b_dn[e]

    out = lax.map(run_block, (row_tok.reshape(n_blk, MOE_BLOCK), blk_exp))
    out = out.reshape(n_rows, D) * row_gate[:, None].astype(out.dtype)
    y = jax.ops.segment_sum(out, row_tok, num_segments=T + 1)[:T]
    return y.reshape(Bsz, L, D)


def setup_inputs(seed: int = 0) -> dict:
    key = jax.random.key(seed)
    ks = jax.random.split(key, 32)
    f32 = jnp.float32

    def nrm(k, shape, scale):
        return jax.random.normal(k, shape, f32) * scale

    def gain(k, shape):
        return 1.0 + 0.05 * jax.random.normal(k, shape, f32)

    dt = jnp.exp(jax.random.uniform(ks[17], (DEPTH, SSM_HEADS), f32, math.log(1e-3), math.log(1e-1)))
    return {
        'x': nrm(ks[0], (BATCH, SEQ, D_MODEL), 1.0),
        'c': nrm(ks[1], (BATCH, D_MODEL), 1.0),
        'rel_bias': nrm(ks[2], (N_BUCKETS, ATTN_HEADS), 0.5),
        'w_ada': nrm(ks[3], (DEPTH, D_MODEL, 6 * D_MODEL), 0.5 * D_MODEL ** -0.5),
        'b_ada': nrm(ks[4], (DEPTH, 6 * D_MODEL), 0.02),
        'norm_mix': gain(ks[5], (DEPTH, D_MODEL)),
        'norm_ffn': gain(ks[6], (DEPTH, D_MODEL)),
        'w_in': nrm(ks[7], (DEPTH, D_MODEL, D_IN), D_MODEL ** -0.5),
        'kv_norm': gain(ks[8], (DEPTH, KV_RANK)),
        'w_kv_up': nrm(ks[9], (DEPTH, KV_RANK, 2 * ATTN_WIDTH), KV_RANK ** -0.5),
        'q_norm': gain(ks[10], (DEPTH, ATTN_HEAD_DIM)),
        'k_norm': gain(ks[11], (DEPTH, ATTN_HEAD_DIM)),
        'idx_k_ln_w': gain(ks[12], (DEPTH, IDX_DIM)),
        'idx_k_ln_b': nrm(ks[13], (DEPTH, IDX_DIM), 0.02),
        'w_attn_o': nrm(ks[14], (DEPTH, ATTN_WIDTH, D_MODEL), ATTN_WIDTH ** -0.5),
        'conv_w': nrm(ks[15], (DEPTH, CONV_WIDTH, CONV_CH), CONV_WIDTH ** -0.5),
        'conv_b': nrm(ks[16], (DEPTH, CONV_CH), 0.02),
        'dt_bias': dt + jnp.log(-jnp.expm1(-dt)),
        'a_log': jnp.log(jax.random.uniform(ks[18], (DEPTH, SSM_HEADS), f32, 1.0, 16.0)),
        'd_skip': gain(ks[19], (DEPTH, SSM_HEADS)),
        'ssm_norm': gain(ks[20], (DEPTH, SSM_INNER)),
        'w_ssm_o': nrm(ks[21], (DEPTH, SSM_INNER, D_MODEL), SSM_INNER ** -0.5),
        'w_out': nrm(ks[22], (DEPTH, D_MODEL, D_MODEL), D_MODEL ** -0.5),
        'w_router': nrm(ks[23], (DEPTH, D_MODEL, N_EXPERTS), D_MODEL ** -0.5),
        'b_router': nrm(ks[24], (DEPTH, N_EXPERTS), 0.01),
        'w_gu': nrm(ks[25], (DEPTH, N_EXPERTS, D_MODEL, 2 * D_EXPERT), D_MODEL ** -0.5),
        'b_gu': nrm(ks[26], (DEPTH, N_EXPERTS, 2 * D_EXPERT), 0.02),
        'w_dn': nrm(ks[27], (DEPTH, N_EXPERTS, D_EXPERT, D_MODEL), D_EXPERT ** -0.5),
        'b_dn': nrm(ks[28], (DEPTH, N_EXPERTS, D_MODEL), 0.02),
    }


def reference(x, c, rel_bias, w_ada, b_ada, norm_mix, norm_ffn, w_in, kv_norm, w_kv_up, q_norm, k_norm,
              idx_k_ln_w, idx_k_ln_b, w_attn_o, conv_w, conv_b, dt_bias, a_log, d_skip, ssm_norm, w_ssm_o,
              w_out, w_router, b_router, w_gu, b_gu, w_dn, b_dn):
    Bsz, L, _ = x.shape
    cond = jax.nn.silu(c)
    for l in range(DEPTH):
        mod = cond @ w_ada[l] + b_ada[l]
        sh_m, sc_m, g_m, sh_f, sc_f, g_f = jnp.split(mod[:, None, :], 6, axis=-1)
        h = _rms(x, norm_mix[l]) * (1.0 + sc_m) + sh_m
        proj = h @ w_in[l]
        q, kv_lat, q_i, k_i, w_i, z, xbc, dt_raw, gate_logits = _split(proj)
        q = _rms(q.reshape(Bsz, L, ATTN_HEADS, ATTN_HEAD_DIM), q_norm[l])
        kv = _rms(kv_lat, kv_norm[l]) @ w_kv_up[l]
        k, v = jnp.split(kv, 2, axis=-1)
        k = _rms(k.reshape(Bsz, L, ATTN_HEADS, ATTN_HEAD_DIM), k_norm[l])
        v = v.reshape(Bsz, L, ATTN_HEADS, ATTN_HEAD_DIM)
        q_i = q_i.reshape(Bsz, L, IDX_HEADS, IDX_DIM) * (IDX_DIM ** -0.5)
        w_i = w_i * (IDX_HEADS ** -0.5)
        k_i = _layernorm(k_i, idx_k_ln_w[l], idx_k_ln_b[l])
        y_attn = _dsa_attention(q, k, v, q_i, w_i, k_i, rel_bias) @ w_attn_o[l]
        y_ssd = _mamba2_ssd(z, xbc, dt_raw, conv_w[l], conv_b[l], dt_bias[l], a_log[l], d_skip[l], ssm_norm[l]) @ w_ssm_o[l]
        g_attn, g_ssd = jnp.split(gate_logits, 2, axis=-1)
        mixed = jax.nn.sigmoid(g_attn) * y_attn + jax.nn.sigmoid(g_ssd) * y_ssd
        x = x + g_m * (mixed @ w_out[l])
        h = _rms(x, norm_ffn[l]) * (1.0 + sc_f) + sh_f
        x = x + g_f * _moe(h, w_router[l], b_router[l], w_gu[l], b_gu[l], w_dn[l], b_dn[l])
    return x
```

```python
import contextlib
import numpy as np
import concourse.bass as bass
import concourse.mybir as mybir
from concourse.bass_utils import run_bass_kernel_spmd

F32 = mybir.dt.float32
BF16 = mybir.dt.bfloat16
I32 = mybir.dt.int32
U32 = mybir.dt.uint32
AF = mybir.ActivationFunctionType
ALU = mybir.AluOpType
AX = mybir.AxisListType

D = 1024
L = 4096
NT = L // 128
DIN = 5976
EPS = 1e-6
NEG = -1.0e30

EPOCH = 20000
NDSEM = 10


class Buf:
    __slots__ = ("name", "w", "r")

    def __init__(self, name=""):
        self.name = name
        self.w = None
        self.r = []


class Op:
    __slots__ = ("eng", "fn", "deps", "dma", "sig", "tick", "slot", "dval", "idx", "grp")


class Sched:
    ENGS = ("pe", "act", "dve", "pool", "sp")

    def __init__(self, nc):
        self.nc = nc
        self.ops = []
        self.ntick = {e: 0 for e in self.ENGS}
        self.ndma = {e: 0 for e in self.ENGS}
        self.csem = {e: [] for e in self.ENGS}
        self.dsem = {}
        self.bufs = []
        self.nphase = 0
        self.group = None
        self.regs = {}

    def buf(self, name=""):
        b = Buf(name)
        self.bufs.append(b)
        return b

    def add(self, eng, fn, reads=(), writes=(), dma=False):
        op = Op()
        op.eng = eng
        op.fn = fn
        op.dma = dma
        op.sig = False
        op.tick = 0
        op.idx = len(self.ops)
        op.grp = self.group
        deps = set()
        for b in reads:
            if b.w is not None:
                deps.add(b.w)
        for b in writes:
            if b.w is not None:
                deps.add(b.w)
            for r in b.r:
                deps.add(r)
        for b in reads:
            b.r.append(op.idx)
        for b in writes:
            b.w = op.idx
            b.r = []
        deps.discard(op.idx)
        op.deps = deps
        self.ops.append(op)
        return op

    def pe(self, fn, reads=(), writes=()):
        return self.add("pe", fn, reads, writes)

    def act(self, fn, reads=(), writes=()):
        return self.add("act", fn, reads, writes)

    def dve(self, fn, reads=(), writes=()):
        return self.add("dve", fn, reads, writes)

    def pool(self, fn, reads=(), writes=()):
        return self.add("pool", fn, reads, writes)

    def dma(self, fn, reads=(), writes=(), q="sp"):
        return self.add(q, fn, reads, writes, dma=True)

    def regload(self, ap, reads=()):
        for en in self.ENGS:
            self.add(en, (lambda e, en=en: e.reg_load(self.regs[en], ap)), reads, ())

    def emit(self):
        nc = self.nc
        ops = self.ops
        for op in ops:
            for d in op.deps:
                ops[d].sig = True
        last_dma = {}
        for op in ops:
            if op.dma:
                k = self.ndma[op.eng]
                op.slot = k % NDSEM
                op.dval = 16 * (k // NDSEM + 1)
                self.ndma[op.eng] = k + 1
                last_dma[(op.eng, op.slot)] = op
            elif op.sig:
                self.ntick[op.eng] += 1
                op.tick = self.ntick[op.eng]
        for e in self.ENGS:
            need = (self.ntick[e] + EPOCH - 1) // EPOCH
            while len(self.csem[e]) < max(need, 1):
                self.csem[e].append(nc.alloc_semaphore(f"c_{e}_{len(self.csem[e])}"))
            if self.ndma[e] and e not in self.dsem:
                self.dsem[e] = [nc.alloc_semaphore(f"d_{e}_{i}") for i in range(NDSEM)]
        csem, dsem = self.csem, self.dsem
        per_eng = {e: [op for op in ops if op.eng == e] for e in self.ENGS}

        def run_engine(ename, eng):
            waited = {e: 0 for e in self.ENGS}
            dwaited = {}

            def wait_for(d):
                if d.dma:
                    key = (d.eng, d.slot)
                    if dwaited.get(key, 0) >= d.dval:
                        return
                    eng.wait_ge(dsem[d.eng][d.slot], d.dval)
                    dwaited[key] = d.dval
                else:
                    if waited[d.eng] >= d.tick:
                        return
                    if d.eng == ename and ename == "pe":
                        return
                    t = d.tick - 1
                    eng.wait_ge(csem[d.eng][t // EPOCH], t % EPOCH + 1)
                    waited[d.eng] = d.tick

            def emit_op(op):
                best = {}
                for di in op.deps:
                    d = ops[di]
                    key = ("d", d.eng, d.slot) if d.dma else ("c", d.eng)
                    v = d.dval if d.dma else d.tick
                    if key not in best or v > best[key][0]:
                        best[key] = (v, d)
                for key in sorted(best):
                    wait_for(best[key][1])
                if op.dma:
                    if op.dval > 16:
                        key = (ename, op.slot)
                        if dwaited.get(key, 0) < op.dval - 16:
                            eng.wait_ge(dsem[ename][op.slot], op.dval - 16)
                            dwaited[key] = op.dval - 16
                    ins = op.fn(eng)
                    ins.then_inc(dsem[ename][op.slot], 16)
                else:
                    ins = op.fn(eng)
                    if op.sig:
                        t = op.tick - 1
                        ins.then_inc(csem[ename][t // EPOCH], 1)

            def compensate(gops):
                sigs = [o for o in gops if (not o.dma) and o.sig]
                if sigs:
                    t0_ = sigs[0].tick - 1
                    if t0_ >= 1:
                        eng.wait_ge(csem[ename][(t0_ - 1) // EPOCH], (t0_ - 1) % EPOCH + 1)
                    per_ep = {}
                    for o in sigs:
                        ep = (o.tick - 1) // EPOCH
                        per_ep[ep] = per_ep.get(ep, 0) + 1
                    for ep in sorted(per_ep):
                        eng.sem_inc(csem[ename][ep], per_ep[ep])
                for o in gops:
                    if o.dma:
                        if o.dval > 16:
                            eng.wait_ge(dsem[ename][o.slot], o.dval - 16)
                        eng.sem_inc(dsem[ename][o.slot], 16)

            lst = per_eng[ename]
            if any(o.grp is not None for o in ops) and ename not in self.regs:
                self.regs[ename] = eng.alloc_register("pred_" + ename)

            def emit_run(run, level):
                i = 0
                while i < len(run):
                    op = run[i]
                    if op.grp is None or len(op.grp) <= level:
                        emit_op(op)
                        i += 1
                        continue
                    g = op.grp[level]
                    j = i
                    while j < len(run) and run[j].grp is not None and len(run[j].grp) > level and run[j].grp[level] == g:
                        j += 1
                    sv_w, sv_d = dict(waited), dict(dwaited)
                    with eng.If_cmp(self.regs[ename], g[1], "IS_GT"):
                        emit_run(run[i:j], level + 1)
                    waited.clear(); waited.update(sv_w)
                    dwaited.clear(); dwaited.update(sv_d)
                    with eng.Else():
                        compensate(run[i:j])
                    i = j

            emit_run(lst, 0)
            if ename == "sp":
                for d in last_dma.values():
                    wait_for(d)

        with nc.Block() as block:
            @block.sync
            def _(e):
                run_engine("sp", e)

            @block.scalar
            def _(e):
                run_engine("act", e)

            @block.vector
            def _(e):
                run_engine("dve", e)

            @block.gpsimd
            def _(e):
                run_engine("pool", e)

            @block.tensor
            def _(e):
                run_engine("pe", e)
        n = len(ops)
        self.ops = []
        for b in self.bufs:
            b.w = None
            b.r = []
        self.bufs = [b for b in self.bufs if getattr(b, "name", "").startswith("P:")]
        self.nphase += 1
        return n


class T:
    __slots__ = ("t", "b")

    def __init__(self, t, b):
        self.t = t
        self.b = b


class K:
    def __init__(self, nc):
        self.nc = nc
        self.S = Sched(nc)
        self.st = None
        self.uid = 0

    def phase(self):
        self.st = contextlib.ExitStack()
        return self.st

    @contextlib.contextmanager
    def sub(self):
        outer = self.st
        with contextlib.ExitStack() as inner:
            self.st = inner
            try:
                yield inner
            finally:
                self.st = outer

    def sb(self, shape, dt, name=None, persist=False):
        self.uid += 1
        t = self.st.enter_context(self.nc.sbuf_tensor(f"{name or 's'}_{self.uid}", list(shape), dt))
        return T(t, self.S.buf("P:x" if persist else ""))

    def ps(self, shape=(128, 512), dt=F32, name=None):
        self.uid += 1
        t = self.st.enter_context(self.nc.psum_tensor(f"{name or 'p'}_{self.uid}", list(shape), dt))
        return T(t, self.S.buf())

    def psb(self, shape, dt, name):
        t = self.nc.alloc_sbuf_tensor(name, list(shape), dt)
        return T(t, self.S.buf("P:" + name))

    def dram(self, name, shape, dt, kind="Internal"):
        t = self.nc.dram_tensor(name, list(shape), dt, kind=kind).ap()
        return T(t, self.S.buf("P:" + name))

    def _b(self, ts):
        return [t.b for t in ts]

    def act(self, out, in_, func, R, W, **kw):
        self.S.act(lambda e: e.activation(out=out, in_=in_, func=func, **kw), self._b(R), self._b(W))

    def ts(self, out, in0, s1, s2, op0, op1, R, W, eng="dve", **kw):
        if op1 is None:
            fn = lambda e: e.tensor_scalar(out=out, in0=in0, scalar1=s1, scalar2=None, op0=op0, **kw)
        else:
            fn = lambda e: e.tensor_scalar(out=out, in0=in0, scalar1=s1, scalar2=s2, op0=op0, op1=op1, **kw)
        self.S.add(eng, fn, self._b(R), self._b(W))

    def tt(self, out, in0, in1, op, R, W, eng="dve"):
        self.S.add(eng, lambda e: e.tensor_tensor(out=out, in0=in0, in1=in1, op=op), self._b(R), self._b(W))

    def stt(self, out, in0, scalar, in1, op0, op1, R, W, **kw):
        self.S.dve(lambda e: e.scalar_tensor_tensor(out=out, in0=in0, scalar=scalar, in1=in1, op0=op0, op1=op1, **kw),
                   self._b(R), self._b(W))

    def mm(self, out, lhsT, rhs, start, stop, R, W):
        self.S.pe(lambda e: e.matmul(out, lhsT=lhsT, rhs=rhs, start=start, stop=stop), self._b(R), self._b(W))

    def tr(self, out, in_, ident, R, W):
        self.S.pe(lambda e: e.transpose(out=out, in_=in_, identity=ident), self._b(R), self._b(W))

    def cp(self, out, in_, R, W, eng="dve"):
        if eng == "act":
            self.S.add(eng, lambda e: e.activation(out=out, in_=in_, func=AF.Copy), self._b(R), self._b(W))
        else:
            self.S.add(eng, lambda e: e.tensor_copy(out=out, in_=in_), self._b(R), self._b(W))

    def ms(self, ap, val, W, eng="pool"):
        self.S.add(eng, lambda e: e.memset(ap, val), [], self._b(W))

    def rcp(self, out, in_, R, W):
        self.S.dve(lambda e: e.reciprocal(out=out, in_=in_), self._b(R), self._b(W))

    def dma(self, out, in_, R, W, q="sp", **kw):
        return self.S.dma(lambda e: e.dma_start(out=out, in_=in_, **kw), self._b(R), self._b(W), q=q)

    def rot(self, tiles):
        st = {"i": -1}

        def nxt():
            st["i"] += 1
            return tiles[st["i"] % len(tiles)]
        return nxt


C_Q, C_KV, C_QI, C_KI, C_WI, C_Z, C_XBC, C_DT, C_G = 0, 512, 768, 1280, 1344, 1352, 2376, 3912, 3928

CP_NM = 0
CP_NF = 8
CP_KVN = 16
CP_QN = 18
CP_KN = 19
CP_LNW = 20
CP_LNB = 21
CP_CONVW = 22
CP_CONVB = 70
CP_BGU = 82
NCOLP = 82 + 512
RP_DTB, RP_ALOG, RP_DSK, RP_BR = 0, 16, 32, 48
NROWP = 112


def emit_phase_A(k, io):
    S = k.S
    with k.phase():
        cc = k.sb([128, 8], F32)
        cond = k.sb([128, 8], F32)
        k.dma(cc.t[:], io["ccols"].t, [], [cc])
        k.act(cond.t[:], cc.t[:], AF.Silu, [cc], [cond])
        wa = k.rot([k.sb([128, 8, 1536], F32, "wa") for _ in range(2)])
        brow = k.sb([1, 6144], F32)
        mrow = k.sb([1, 6144], F32)
        pss = k.rot([k.ps() for _ in range(6)])
        for l in range(2):
            k.dma(brow.t[:], io["b_ada"].t[l:l + 1, :], [], [brow])
            for g in range(4):
                w = wa()
                src = io["w_ada"].t[l].rearrange("(k p) n -> p k n", p=128)[:, :, g * 1536:(g + 1) * 1536]
                k.dma(w.t[:], src, [], [w])
                for j in range(3):
                    p = pss()
                    for kk in range(8):
                        k.mm(p.t[0:1, :], cond.t[:, kk:kk + 1], w.t[:, kk, j * 512:(j + 1) * 512], kk == 0, kk == 7,
                             [cond, w], [p])
                    c0 = g * 1536 + j * 512
                    k.tt(mrow.t[0:1, c0:c0 + 512], p.t[0:1, :], brow.t[0:1, c0:c0 + 512], ALU.add, [p, brow], [mrow])
            k.dma(io["modD"].t[l:l + 1, :], mrow.t[:], [mrow], [io["modD"]])
        for l in range(2):
            mc = k.sb([128, 48], F32)
            k.dma(mc.t[:], io["modD"].t[l].rearrange("(j p) -> p j", p=128), [io["modD"]], [mc],
                  allow_slow_non_contiguous=True)
            cp = io["colp_sb"][l]
            gs = io["GS"][l]
            k.stt(gs.t[:, 0:8], mc.t[:, 8:16], 1.0, cp.t[:, CP_NM:CP_NM + 8], ALU.add, ALU.mult, [mc, cp], [gs])
            k.cp(gs.t[:, 8:16], mc.t[:, 0:8], [mc], [gs])
            k.stt(gs.t[:, 16:24], mc.t[:, 32:40], 1.0, cp.t[:, CP_NF:CP_NF + 8], ALU.add, ALU.mult, [mc, cp], [gs])
            k.cp(gs.t[:, 24:32], mc.t[:, 24:32], [mc], [gs])
        S.emit()


def rms_rstd(k, ss, tmp):
    k.ts(tmp.t[:, 1:2], ss.t[:, 0:1], 1.0 / D, EPS, ALU.mult, ALU.add, [ss], [tmp])
    k.act(tmp.t[:, 2:3], tmp.t[:, 1:2], AF.Sqrt, [tmp], [tmp])
    k.rcp(tmp.t[:, 0:1], tmp.t[:, 2:3], [tmp], [tmp])


def norm_tile(k, x_, xs_, sq, ss, tmp):
    k.act(sq.t[:], x_.t[:], AF.Square, [x_], [sq, ss], accum_out=ss.t[:, 0:1])
    rms_rstd(k, ss, tmp)
    k.act(xs_.t[:], x_.t[:], AF.Copy, [x_, tmp], [xs_], scale=tmp.t[:, 0:1])


def emit_phase_B(k, io, l, xin):
    S = k.S
    with k.phase():
        gs = io["GS"][l]
        cp = io["colp_sb"][l]
        rp = io["rowp_sb"][l]
        ident = io["ident"]
        identb = io["identb"]
        BD = io["bd64"]
        ON256 = io["on256"]
        WB = k.sb([128, 8, DIN], BF16, "WB")
        stg = k.rot([k.sb([128, 1494], F32, "stg") for _ in range(2)])
        w_in = io["w_in"].t[l].rearrange("(k p) n -> p k n", p=128)
        for kk in range(8):
            for c in range(4):
                s = stg()
                c0 = c * 1494
                k.dma(s.t[:], w_in[:, kk, c0:c0 + 1494], [], [s])
                k.cp(WB.t[:, kk, c0:c0 + 1494], s.t[:], [s], [WB], eng=("act" if c % 2 == 0 else "dve"))
        WKV = k.sb([128, 2, 1024], BF16, "WKV")
        w_kv = io["w_kv_up"].t[l].rearrange("(k p) n -> p k n", p=128)
        for kk in range(2):
            s = stg()
            k.dma(s.t[:, 0:1024], w_kv[:, kk, :], [], [s])
            k.cp(WKV.t[:, kk, :], s.t[:, 0:1024], [s], [WKV], eng="pool")
        aB = k.sb([128, 16], F32)
        k.act(aB.t[:], rp.t[:, RP_ALOG:RP_ALOG + 16], AF.Exp, [rp], [aB])
        k.ts(aB.t[:], aB.t[:], -1.0, None, ALU.mult, None, [aB], [aB])

        xt = k.rot([k.sb([128, D], F32, "xt") for _ in range(2)])
        xs = k.rot([k.sb([128, D], F32, "xs") for _ in range(2)])
        sq = k.sb([128, D], F32, "sqj")
        ssb = k.rot([k.sb([128, 4], F32) for _ in range(2)])
        tmpb = k.rot([k.sb([128, 4], F32) for _ in range(2)])
        hTs = k.rot([k.sb([128, 8, 512], BF16, "hT") for _ in range(2)])
        tp = [k.ps() for _ in range(2)]
        nmm = k.rot([k.ps() for _ in range(3)])
        naux = k.rot([k.ps() for _ in range(2)])
        pt = k.ps([128, 512], BF16, "ptb")
        xc = k.sb([128, 12, 515], F32, "xc")
        k.ms(xc.t[:, :, 0:3], 0.0, [xc])
        nf = k.rot([k.sb([128, 512], F32, "wkf") for _ in range(8)])
        nb = k.rot([k.sb([128, 512], BF16, "wkb") for _ in range(6)])
        kvn = k.sb([128, 2, 512], BF16, "kvn")

        def proj_fm(p, c0, m, h):
            for kk in range(8):
                k.mm(p.t[0:m, :], WB.t[:, kk, c0:c0 + m], h.t[:, kk, :], kk == 0, kk == 7, [WB, h], [p])

        def rstd_from_ms(a, m=128):
            r = nf()
            k.act(r.t[0:m, :], a.t[0:m, :], AF.Ln, [a], [r], bias=io["epsc"].t[0:m, 0:1])
            k.act(r.t[0:m, :], r.t[0:m, :], AF.Exp, [r], [r], scale=-0.5)
            return r

        def head_rms_store(p, gcol, dst, r0, t0):
            qf = nf()
            k.act(qf.t[:], p.t[:], AF.Copy, [p], [qf])
            q2 = nf()
            k.tt(q2.t[:], qf.t[:], qf.t[:], ALU.mult, [qf], [q2])
            a = naux()
            k.mm(a.t[:], BD.t[:], q2.t[:], True, True, [BD, q2], [a])
            r = rstd_from_ms(a)
            o = nb()
            k.stt(o.t[:], qf.t[:], gcol, r.t[:], ALU.mult, ALU.mult, [qf, r, cp], [o])
            k.dma(dst.t[r0:r0 + 128, t0:t0 + 512], o.t[:], [o], [dst])

        def prep(tb):
            h = hTs()
            for tt in range(4):
                ti = tb * 4 + tt
                x_, xs_, ss, tmp = xt(), xs(), ssb(), tmpb()
                k.dma(x_.t[:], xin.t[ti * 128:(ti + 1) * 128, :], [xin], [x_])
                norm_tile(k, x_, xs_, sq, ss, tmp)
                for half in range(2):
                    p = tp[half]
                    for j in range(4):
                        kk = half * 4 + j
                        k.tr(p.t[:, j * 128:(j + 1) * 128], xs_.t[:, kk * 128:(kk + 1) * 128], ident.t[:],
                             [xs_, ident], [p])
                    for j in range(4):
                        kk = half * 4 + j
                        k.ts(h.t[:, kk, tt * 128:(tt + 1) * 128], p.t[:, j * 128:(j + 1) * 128],
                             gs.t[:, kk:kk + 1], gs.t[:, 8 + kk:9 + kk], ALU.mult, ALU.add, [p, gs], [h])
            return h

        def proj(tb, h):
            t0 = tb * 512
            for j in range(4):
                p = nmm()
                proj_fm(p, C_Q + j * 128, 128, h)
                head_rms_store(p, cp.t[:, CP_QN:CP_QN + 1], io["qT"], j * 128, t0)
            kvf = [nf(), nf()]
            kvq = [nf(), nf()]
            for j in range(2):
                p = nmm()
                proj_fm(p, C_KV + j * 128, 128, h)
                k.act(kvf[j].t[:], p.t[:], AF.Copy, [p], [kvf[j]])
                k.tt(kvq[j].t[:], kvf[j].t[:], kvf[j].t[:], ALU.mult, [kvf[j]], [kvq[j]])
            a = naux()
            for j in range(2):
                k.mm(a.t[:], ON256.t[:], kvq[j].t[:], j == 0, j == 1, [ON256, kvq[j]], [a])
            r = rstd_from_ms(a)
            for j in range(2):
                k.stt(kvn.t[:, j, :], kvf[j].t[:], cp.t[:, CP_KVN + j:CP_KVN + j + 1], r.t[:], ALU.mult, ALU.mult,
                      [kvf[j], r, cp], [kvn])
            for j in range(4):
                p = nmm()
                for kk in range(2):
                    k.mm(p.t[:], WKV.t[:, kk, j * 128:(j + 1) * 128], kvn.t[:, kk, :], kk == 0, kk == 1, [WKV, kvn], [p])
                head_rms_store(p, cp.t[:, CP_KN:CP_KN + 1], io["kT"], j * 128, t0)
            for tt in range(4):
                p = nmm()
                for kk in range(2):
                    k.mm(p.t[:], kvn.t[:, kk, tt * 128:(tt + 1) * 128], WKV.t[:, kk, 512:1024], kk == 0, kk == 1,
                         [WKV, kvn], [p])
                o = nb()
                k.act(o.t[:], p.t[:], AF.Copy, [p], [o])
                k.dma(io["v"].t[t0 + tt * 128:t0 + (tt + 1) * 128, :], o.t[:], [o], [io["v"]])
            for j in range(4):
                p = nmm()
                proj_fm(p, C_QI + j * 128, 128, h)
                o = nb()
                k.act(o.t[:], p.t[:], AF.Copy, [p], [o], scale=0.125)
                k.dma(io["qiT"].t[j * 128:(j + 1) * 128, t0:t0 + 512], o.t[:], [o], [io["qiT"]])
            p = nmm()
            proj_fm(p, C_KI, 64, h)
            kf = nf()
            k.act(kf.t[0:64, :], p.t[0:64, :], AF.Copy, [p], [kf])
            a = naux()
            k.mm(a.t[0:64, :], BD.t[0:64, 0:64], kf.t[0:64, :], True, True, [BD, kf], [a])
            cen = nf()
            k.tt(cen.t[0:64, :], kf.t[0:64, :], a.t[0:64, :], ALU.subtract, [kf, a], [cen])
            c2 = nf()
            k.tt(c2.t[0:64, :], cen.t[0:64, :], cen.t[0:64, :], ALU.mult, [cen], [c2])
            a2 = naux()
            k.mm(a2.t[0:64, :], BD.t[0:64, 0:64], c2.t[0:64, :], True, True, [BD, c2], [a2])
            r = rstd_from_ms(a2, 64)
            k.tt(cen.t[0:64, :], cen.t[0:64, :], r.t[0:64, :], ALU.mult, [cen, r], [cen])
            o = nb()
            k.ts(o.t[0:64, :], cen.t[0:64, :], cp.t[0:64, CP_LNW:CP_LNW + 1], cp.t[0:64, CP_LNB:CP_LNB + 1],
                 ALU.mult, ALU.add, [cen, cp], [o])
            k.dma(io["kiT"].t[0:64, t0:t0 + 512], o.t[0:64, :], [o], [io["kiT"]])
            for tt in range(4):
                tok = slice(t0 + tt * 128, t0 + (tt + 1) * 128)
                hs = slice(tt * 128, (tt + 1) * 128)
                p = nmm()
                for kk in range(8):
                    k.mm(p.t[:, 0:8], h.t[:, kk, hs], WB.t[:, kk, C_WI:C_WI + 8], kk == 0, kk == 7, [WB, h], [p])
                for kk in range(8):
                    k.mm(p.t[:, 16:32], h.t[:, kk, hs], WB.t[:, kk, C_DT:C_DT + 16], kk == 0, kk == 7, [WB, h], [p])
                sm = nf()
                k.act(sm.t[:, 0:8], p.t[:, 0:8], AF.Copy, [p], [sm], scale=float(8 ** -0.5))
                k.dma(io["wi"].t[tok, :], sm.t[:, 0:8], [sm], [io["wi"]])
                k.tt(sm.t[:, 16:32], p.t[:, 16:32], rp.t[:, RP_DTB:RP_DTB + 16], ALU.add, [p, rp], [sm])
                k.act(sm.t[:, 16:32], sm.t[:, 16:32], AF.Exp, [sm], [sm])
                k.act(sm.t[:, 32:48], sm.t[:, 16:32], AF.Ln, [sm], [sm], bias=io["onec"].t[:, 0:1])
                k.tt(sm.t[:, 48:64], sm.t[:, 32:48], aB.t[:], ALU.mult, [sm, aB], [sm])
                k.dma(io["dt"].t[tok, :], sm.t[:, 32:64], [sm], [io["dt"]])
                for half in range(2):
                    p = nmm()
                    for kk in range(8):
                        k.mm(p.t[:], h.t[:, kk, hs], WB.t[:, kk, C_Z + half * 512:C_Z + (half + 1) * 512], kk == 0,
                             kk == 7, [WB, h], [p])
                    o = nb()
                    k.act(o.t[:], p.t[:], AF.Copy, [p], [o])
                    k.dma(io["z"].t[tok, half * 512:(half + 1) * 512], o.t[:], [o], [io["z"]])
            for j in range(16):
                p = nmm()
                proj_fm(p, C_G + j * 128, 128, h)
                o = nb()
                k.act(o.t[:], p.t[:], AF.Sigmoid, [p], [o])
                k.dma(io["gT"].t[j * 128:(j + 1) * 128, t0:t0 + 512], o.t[:], [o], [io["gT"]])
            for ct in range(12):
                p = nmm()
                proj_fm(p, C_XBC + ct * 128, 128, h)
                k.act(xc.t[:, ct, 3:515], p.t[:], AF.Copy, [p], [xc])
                acc = nf()
                w0 = CP_CONVW + ct * 4
                k.ts(acc.t[:], xc.t[:, ct, 3:515], cp.t[:, w0 + 3:w0 + 4], cp.t[:, CP_CONVB + ct:CP_CONVB + ct + 1],
                     ALU.mult, ALU.add, [xc, cp], [acc])
                for kw in range(3):
                    k.stt(acc.t[:], xc.t[:, ct, kw:kw + 512], cp.t[:, w0 + kw:w0 + kw + 1], acc.t[:], ALU.mult, ALU.add,
                          [xc, cp, acc], [acc])
                k.cp(xc.t[:, ct, 0:3], xc.t[:, ct, 512:515], [xc], [xc], eng="pool")
                o = nb()
                k.act(o.t[:], acc.t[:], AF.Silu, [acc], [o])
                if ct >= 8:
                    dst = io["BT"] if ct < 10 else io["CT"]
                    r0 = (ct - 8) * 128 if ct < 10 else (ct - 10) * 128
                    k.dma(dst.t[r0:r0 + 128, t0:t0 + 512], o.t[:], [o], [dst])
                if ct < 10:
                    for tt in range(4):
                        k.tr(pt.t[:, tt * 128:(tt + 1) * 128], o.t[:, tt * 128:(tt + 1) * 128], identb.t[:],
                             [o, identb], [pt])
                    o2 = nb()
                    k.act(o2.t[:], pt.t[:], AF.Copy, [pt], [o2])
                    if ct < 8:
                        dstT, dv = io["xs"], io["xs"].t[t0:t0 + 512, ct * 128:(ct + 1) * 128]
                    else:
                        dstT, dv = io["Btok"], io["Btok"].t[t0:t0 + 512, (ct - 8) * 128:(ct - 7) * 128]
                    k.dma(dv.rearrange("(a p) c -> p a c", p=128), o2.t[:].rearrange("p (a c) -> p a c", a=4),
                          [o2], [dstT])

        hcur = prep(0)
        for tb in range(8):
            hnext = prep(tb + 1) if tb + 1 < 8 else None
            proj(tb, hcur)
            hcur = hnext
        S.emit()


TOPK_ITERS = 28
TOPK_RANGE = 128.0
CC_ID, CC_BD, CC_ON, CC_EPS, CC_ONE, CC_J, CC_CNEG, CC_OHM, CC_VM = 0, 128, 256, 384, 385, 512, 640, 768, 1152


def emit_phase_C0(k, io):
    S = k.S
    with k.phase():
        cst = k.sb([128, 2048], F32)
        k.dma(cst.t[:], io["consts"].t, [], [cst])
        rb = k.sb([32, 8], F32)
        k.dma(rb.t[:], io["rel_bias"].t, [], [rb])
        p = k.ps()
        k.mm(p.t[0:8, 0:384], rb.t[:, :], cst.t[0:32, CC_OHM:CC_OHM + 384], True, True, [rb, cst], [p])
        g = k.sb([8, 384], F32)
        k.act(g.t[:], p.t[0:8, 0:384], AF.Exp, [p], [g])
        k.tt(g.t[:], g.t[:], cst.t[0:8, CC_VM:CC_VM + 384], ALU.mult, [g, cst], [g])
        k.dma(io["Gd"].t, g.t[:], [g], [io["Gd"]])
        xh = k.rot([k.sb([128, 128], F32, "xh") for _ in range(2)])
        pp = k.rot([k.ps() for _ in range(2)])
        for h in range(8):
            for which in range(2):
                x = xh()
                src = bass.AP(io["Gd"].t.tensor, h * 384 + which * 128, [[1, 128], [1, 128]])
                k.dma(x.t[:], src, [io["Gd"]], [x])
                q = pp()
                k.mm(q.t[:, 0:128], cst.t[:, CC_J:CC_J + 128], x.t[:], True, True, [cst, x], [q])
                dst = io["EBd"] if which == 0 else io["EBo"]
                k.act(dst.t[:, h, :], q.t[:, 0:128], AF.Copy, [q], [dst])
        S.emit()


def emit_phase_C(k, io, l):
    S = k.S
    with k.phase():
        identb = io["identb"]
        EBd, EBo = io["EBd"], io["EBo"]
        kT = k.sb([128, 4, L], BF16, "kT")
        for j in range(4):
            k.dma(kT.t[:, j, :], io["kT"].t[j * 128:(j + 1) * 128, :], [io["kT"]], [kT])
        kiT2 = k.sb([128, L], BF16, "kiT2")
        k.dma(kiT2.t[0:64, :], io["kiT"].t, [io["kiT"]], [kiT2])
        k.dma(kiT2.t[64:128, :], io["kiT"].t, [io["kiT"]], [kiT2])
        VP = k.sb([128, NT, 8, 65], BF16, "VP")
        k.ms(VP.t[:], 1.0, [VP])
        for a in range(NT):
            k.dma(VP.t[:, a, :, 0:64], io["v"].t[a * 128:(a + 1) * 128, :].rearrange("p (h d) -> p h d", d=64),
                  [io["v"]], [VP])
        cneg = k.sb([128, 128], F32)
        k.dma(cneg.t[:], io["consts"].t[:, CC_CNEG:CC_CNEG + 128], [], [cneg])
        sc = k.sb([128, L], F32, "sc")
        wrk = k.sb([128, L], BF16, "wrk")
        thrr = k.rot([k.sb([128, 1], F32, "thr") for _ in range(2)])
        cntr = k.rot([k.sb([128, 1], F32, "cnt") for _ in range(2)])
        tqr = k.rot([k.sb([128, 1], F32, "tq") for _ in range(2)])
        mskr = k.rot([k.sb([128, L], BF16, "msk") for _ in range(2)])
        mTr = k.rot([k.sb([128, NT, 128], BF16, "mT") for _ in range(2)])
        m8 = k.rot([k.sb([128, 8], F32) for _ in range(2)])
        qis = k.rot([k.sb([128, 4, 128], BF16, "qi") for _ in range(2)])
        qts = k.rot([k.sb([128, 4, 128], BF16, "qt") for _ in range(2)])
        wis = k.rot([k.sb([128, 8], F32, "wi") for _ in range(2)])
        rls = k.rot([k.sb([128, 512], F32, "rl") for _ in range(3)])
        es = k.rot([k.sb([128, 512], BF16, "E") for _ in range(3)])
        pts = k.rot([k.sb([128, 512], BF16, "P") for _ in range(3)])
        att = k.rot([k.sb([128, 512], BF16, "att") for _ in range(2)])
        attT = k.rot([k.sb([128, 512], BF16, "attT") for _ in range(2)])
        rden = k.rot([k.sb([128, 8], F32, "rden") for _ in range(2)])
        pi = k.rot([k.ps() for _ in range(2)])
        pq = k.rot([k.ps() for _ in range(2)])
        pm = k.ps([128, 512], BF16, "pm")
        po = [k.ps([128, 4, 65], F32, "po") for _ in range(2)]
        pa = k.ps([128, 512], BF16, "pa")

        def stage1(qi):
            nk = qi + 1
            nkeys = nk * 128
            qs = slice(qi * 128, (qi + 1) * 128)
            qi_t, q_t, wi_t = qis(), qts(), wis()
            msk, mT = mskr(), mTr()
            k.dma(qi_t.t[:], io["qiT"].t[:, qs].rearrange("(j p) q -> p j q", p=128), [io["qiT"]], [qi_t])
            k.dma(q_t.t[:], io["qT"].t[:, qs].rearrange("(j p) q -> p j q", p=128), [io["qT"]], [q_t])
            k.dma(wi_t.t[:], io["wi"].t[qs, :], [io["wi"]], [wi_t])
            for kc in range((nk + 3) // 4):
                c0 = kc * 512
                wd = min(512, nkeys - c0)
                for hh in range(8):
                    j, r = hh // 2, hh % 2
                    p = pi()
                    k.mm(p.t[:, 0:wd], qi_t.t[r * 64:(r + 1) * 64, j, :], kiT2.t[r * 64:(r + 1) * 64, c0:c0 + wd],
                         True, True, [qi_t, kiT2], [p])
                    rl = rls()
                    k.act(rl.t[:, 0:wd], p.t[:, 0:wd], AF.Relu, [p], [rl])
                    if hh == 0:
                        k.ts(sc.t[:, c0:c0 + wd], rl.t[:, 0:wd], wi_t.t[:, 0:1], None, ALU.mult, None, [rl, wi_t], [sc])
                    else:
                        k.stt(sc.t[:, c0:c0 + wd], rl.t[:, 0:wd], wi_t.t[:, hh:hh + 1], sc.t[:, c0:c0 + wd], ALU.mult,
                              ALU.add, [rl, wi_t, sc], [sc])
            k.tt(sc.t[:, qs], sc.t[:, qs], cneg.t[:], ALU.add, [sc, cneg], [sc])
            if qi >= 2:
                thr, cnt, tq = thrr(), cntr(), tqr()
                k.ms(thr.t[:], 0.0, [thr], eng="dve")
                for it in range(TOPK_ITERS):
                    step = TOPK_RANGE / (2.0 ** (it + 1))
                    k.ts(wrk.t[:, 0:nkeys], sc.t[:, 0:nkeys], thr.t[:, 0:1], 0.0, ALU.is_ge, ALU.add, [sc, thr],
                         [wrk, cnt], accum_out=cnt.t[:, 0:1])
                    k.ts(tq.t[:], cnt.t[:], 255.5, 2.0 * step, ALU.is_ge, ALU.mult, [cnt], [tq])
                    k.stt(thr.t[:], tq.t[:], -step, thr.t[:], ALU.add, ALU.add, [tq, thr], [thr])
                k.ts(thr.t[:], thr.t[:], -2.0 * step, None, ALU.add, None, [thr], [thr])
                k.ts(msk.t[:, 0:nkeys], sc.t[:, 0:nkeys], thr.t[:, 0:1], None, ALU.is_ge, None, [sc, thr], [msk])
            else:
                k.ts(msk.t[:, 0:nkeys], sc.t[:, 0:nkeys], -1.0e29, None, ALU.is_ge, None, [sc], [msk])
            return q_t, msk, mT

        def stage2(qi, q_t, msk, mT):
            nk = qi + 1
            qs = slice(qi * 128, (qi + 1) * 128)
            for g4 in range((nk + 3) // 4):
                n4 = min(4, nk - g4 * 4)
                for a in range(n4):
                    sj = g4 * 4 + a
                    k.tr(pm.t[:, a * 128:(a + 1) * 128], msk.t[:, sj * 128:(sj + 1) * 128], identb.t[:],
                         [msk, identb], [pm])
                k.act(mT.t[:, g4 * 4:g4 * 4 + n4, :], pm.t[:, 0:n4 * 128].rearrange("p (a q) -> p a q", a=n4), AF.Copy,
                      [pm], [mT])
            for hh in range(8):
                j, r = hh // 2, hh % 2
                pod = po[hh // 4]
                for g4 in range((nk + 3) // 4):
                    n4 = min(4, nk - g4 * 4)
                    p = pq()
                    for a in range(n4):
                        sj = g4 * 4 + a
                        k.mm(p.t[:, a * 128:(a + 1) * 128], kT.t[r * 64:(r + 1) * 64, j, sj * 128:(sj + 1) * 128],
                             q_t.t[r * 64:(r + 1) * 64, j, :], True, True, [kT, q_t], [p])
                    e_ = es()
                    k.act(e_.t[:, 0:n4 * 128], p.t[:, 0:n4 * 128], AF.Exp, [p], [e_], scale=0.125)
                    pt = pts()
                    k.tt(pt.t[:, 0:n4 * 128].rearrange("p (a q) -> p a q", a=n4),
                         e_.t[:, 0:n4 * 128].rearrange("p (a q) -> p a q", a=n4), mT.t[:, g4 * 4:g4 * 4 + n4, :],
                         ALU.mult, [e_, mT], [pt], eng="pool")
                    for a in range(n4):
                        sj = g4 * 4 + a
                        if sj == qi:
                            k.tt(pt.t[:, a * 128:(a + 1) * 128], pt.t[:, a * 128:(a + 1) * 128], EBd.t[:, hh, :],
                                 ALU.mult, [pt, EBd], [pt], eng="pool")
                        elif sj == qi - 1:
                            k.tt(pt.t[:, a * 128:(a + 1) * 128], pt.t[:, a * 128:(a + 1) * 128], EBo.t[:, hh, :],
                                 ALU.mult, [pt, EBo], [pt], eng="pool")
                    for a in range(n4):
                        sj = g4 * 4 + a
                        k.mm(pod.t[:, hh % 4, :], pt.t[:, a * 128:(a + 1) * 128], VP.t[:, sj, hh, :], sj == 0,
                             sj == qi, [pt, VP], [pod])
            rd, at, atT = rden(), att(), attT()
            for half in range(2):
                k.rcp(rd.t[:, half * 4:(half + 1) * 4], po[half].t[:, :, 64], [po[half]], [rd])
                k.tt(at.t[:, half * 256:(half + 1) * 256].rearrange("p (h d) -> p h d", h=4), po[half].t[:, :, 0:64],
                     rd.t[:, half * 4:(half + 1) * 4].unsqueeze(2).to_broadcast([128, 4, 64]), ALU.mult,
                     [po[half], rd], [at])
            for j in range(4):
                k.tr(pa.t[:, j * 128:(j + 1) * 128], at.t[:, j * 128:(j + 1) * 128], identb.t[:], [at, identb], [pa])
            k.act(atT.t[:], pa.t[:], AF.Copy, [pa], [atT])
            k.dma(io["attT"].t[:, qs].rearrange("(j p) q -> p j q", p=128), atT.t[:].rearrange("p (j q) -> p j q", j=4),
                  [atT], [io["attT"]])

        nxt = stage1(0)
        for qi in range(NT):
            cur = nxt
            if qi + 1 < NT:
                nxt = stage1(qi + 1)
            stage2(qi, *cur)
        S.emit()


CC_T, CC_SU, CC_ONES = 1536, 1664, 1792


def emit_phase_D(k, io, l):
    S = k.S
    with k.phase():
        identb = io["identb"]
        rp = io["rowp_sb"][l]
        cst = k.sb([128, 384], F32, "cstD")
        k.dma(cst.t[:], io["consts"].t[:, CC_T:CC_T + 384], [], [cst])
        Tm = cst.t[:, 0:128]
        SU = cst.t[:, 128:256]
        ON = cst.t[:, 256:384]
        snB = k.sb([128, D], F32, "snB")
        k.dma(snB.t[:], io["ssm_norm"].t[l:l + 1, :].partition_broadcast(128), [], [snB])
        St = k.sb([128, D], F32, "St")
        Sb = k.sb([128, D], BF16, "Sb")
        k.ms(St.t[:], 0.0, [St])
        k.ms(Sb.t[:], 0.0, [Sb])
        xsr = k.rot([k.sb([128, D], BF16, "xs") for _ in range(2)])
        zr = k.rot([k.sb([128, D], BF16, "z") for _ in range(2)])
        btr = k.rot([k.sb([128, 256], BF16, "bt") for _ in range(2)])
        BTr = k.rot([k.sb([128, 2, 128], BF16, "BT") for _ in range(2)])
        CTr = k.rot([k.sb([128, 2, 128], BF16, "CT") for _ in range(2)])
        dtr = k.rot([k.sb([128, 32], F32, "dt") for _ in range(2)])
        e3r = k.rot([k.sb([128, 64], F32, "e3") for _ in range(2)])
        cbr = k.rot([k.sb([128, 2, 128], F32, "cb") for _ in range(2)])
        lhr = k.rot([k.sb([128, 128], F32, "lh") for _ in range(3)])
        lmr = k.rot([k.sb([128, 128], F32, "lm") for _ in range(3)])
        wtr = k.rot([k.sb([128, 128], BF16, "wt") for _ in range(3)])
        xdr = k.rot([k.sb([128, D], BF16, "xd") for _ in range(2)])
        y1r = k.rot([k.sb([128, D], F32, "y1") for _ in range(2)])
        t1r = k.rot([k.sb([128, D], F32, "t1") for _ in range(2)])
        ynr = k.rot([k.sb([128, D], BF16, "yn") for _ in range(2)])
        yTr = k.rot([k.sb([128, D], BF16, "yT") for _ in range(2)])
        smr = k.rot([k.sb([128, 8], F32, "sm") for _ in range(2)])
        tS = k.sb([128, D], F32, "tS")
        pa = k.ps()
        pseg = k.rot([k.ps() for _ in range(2)])
        py = [k.ps() for _ in range(2)]
        pyo = [k.ps() for _ in range(2)]
        ptr = k.ps([128, 512], BF16, "ptr")

        def b3(ap16):
            return ap16.unsqueeze(2).to_broadcast([128, ap16.shape[1], 64])

        def v3(ap, nh):
            return ap.rearrange("p (h d) -> p h d", h=nh)

        for c in range(NT):
            tok = slice(c * 128, (c + 1) * 128)
            xs_c, z_c, bt_c, BT_c, CT_c, dt_c = xsr(), zr(), btr(), BTr(), CTr(), dtr()
            k.dma(xs_c.t[:], io["xs"].t[tok, :], [io["xs"]], [xs_c])
            k.dma(z_c.t[:], io["z"].t[tok, :], [io["z"]], [z_c])
            k.dma(bt_c.t[:], io["Btok"].t[tok, :], [io["Btok"]], [bt_c])
            k.dma(BT_c.t[:], io["BT"].t[:, tok].rearrange("(g n) t -> n g t", g=2), [io["BT"]], [BT_c])
            k.dma(CT_c.t[:], io["CT"].t[:, tok].rearrange("(g n) t -> n g t", g=2), [io["CT"]], [CT_c])
            k.dma(dt_c.t[:], io["dt"].t[tok, :], [io["dt"]], [dt_c])
            dA = dt_c.t[:, 16:32]
            k.mm(pa.t[:, 0:16], Tm, dA, True, True, [cst, dt_c], [pa])
            k.mm(pa.t[:, 16:32], SU, dA, True, True, [cst, dt_c], [pa])
            k.mm(pa.t[:, 32:48], ON, dA, True, True, [cst, dt_c], [pa])
            e3 = e3r()
            k.act(e3.t[:, 0:48], pa.t[:, 0:48], AF.Exp, [pa], [e3])
            k.tt(e3.t[:, 48:64], e3.t[:, 16:32], dt_c.t[:, 0:16], ALU.mult, [e3, dt_c], [e3])
            cb = cbr()
            for g in range(2):
                k.mm(pa.t[:, 128 + g * 128:256 + g * 128], BT_c.t[:, g, :], CT_c.t[:, g, :], True, True,
                     [BT_c, CT_c], [pa])
            k.tt(cb.t[:], pa.t[:, 128:384].rearrange("p (g i) -> p g i", g=2),
                 Tm.unsqueeze(1).to_broadcast([128, 2, 128]), ALU.mult, [pa, cst], [cb])
            for g in range(2):
                k.mm(pyo[g].t[:], CT_c.t[:, g, :], Sb.t[:, g * 512:(g + 1) * 512], True, True, [CT_c, Sb], [pyo[g]])
            for h in range(16):
                g = h // 8
                lh, lm, wt = lhr(), lmr(), wtr()
                k.ts(lh.t[:], SU, dt_c.t[:, 16 + h:17 + h], None, ALU.mult, None, [cst, dt_c], [lh], eng=("pool" if h % 4 == 3 else "dve"))
                ps = pseg()
                k.mm(ps.t[:, 0:128], lh.t[:], Tm, True, True, [lh, cst], [ps])
                k.act(lm.t[:], ps.t[:, 0:128], AF.Exp, [ps], [lm])
                k.stt(wt.t[:], lm.t[:], dt_c.t[:, h:h + 1], cb.t[:, g, :], ALU.mult, ALU.mult, [lm, dt_c, cb], [wt])
                k.mm(py[g].t[:, (h % 8) * 64:(h % 8 + 1) * 64], wt.t[:], xs_c.t[:, h * 64:(h + 1) * 64], True, True,
                     [wt, xs_c], [py[g]])
            xd = xdr()
            k.tt(v3(xd.t[:], 16), v3(xs_c.t[:], 16), b3(e3.t[:, 48:64]), ALU.mult, [xs_c, e3], [xd], eng="pool")
            for g in range(2):
                ps = pseg()
                k.mm(ps.t[:], bt_c.t[:, g * 128:(g + 1) * 128], xd.t[:, g * 512:(g + 1) * 512], True, True,
                     [bt_c, xd], [ps])
                hs = slice(g * 512, (g + 1) * 512)
                k.tt(v3(tS.t[:, hs], 8), v3(St.t[:, hs], 8), b3(e3.t[:, 32 + g * 8:40 + g * 8]), ALU.mult, [St, e3], [tS])
                k.tt(St.t[:, hs], tS.t[:, hs], ps.t[:], ALU.add, [tS, ps], [St])
            y1, t1 = y1r(), t1r()
            for g in range(2):
                hs = slice(g * 512, (g + 1) * 512)
                k.tt(v3(y1.t[:, hs], 8), v3(pyo[g].t[:], 8), b3(e3.t[:, g * 8:(g + 1) * 8]), ALU.mult, [pyo[g], e3], [y1])
                k.tt(y1.t[:, hs], y1.t[:, hs], py[g].t[:], ALU.add, [y1, py[g]], [y1])
            k.act(Sb.t[:], St.t[:], AF.Copy, [St], [Sb])
            k.tt(v3(t1.t[:], 16), v3(xs_c.t[:], 16), b3(rp.t[:, RP_DSK:RP_DSK + 16]), ALU.mult, [xs_c, rp], [t1],
                 eng="pool")
            k.tt(y1.t[:], y1.t[:], t1.t[:], ALU.add, [y1, t1], [y1])
            k.act(t1.t[:], z_c.t[:], AF.Silu, [z_c, t1], [t1])
            k.tt(y1.t[:], y1.t[:], t1.t[:], ALU.mult, [y1, t1], [y1])
            sm = smr()
            for g in range(2):
                hs = slice(g * 512, (g + 1) * 512)
                k.act(t1.t[:, hs], y1.t[:, hs], AF.Square, [y1, t1], [t1, sm], accum_out=sm.t[:, g:g + 1])
            k.ts(sm.t[:, 2:4], sm.t[:, 0:2], 1.0 / 512, EPS, ALU.mult, ALU.add, [sm], [sm])
            k.act(sm.t[:, 4:6], sm.t[:, 2:4], AF.Sqrt, [sm], [sm])
            k.rcp(sm.t[:, 6:8], sm.t[:, 4:6], [sm], [sm])
            yn, yT = ynr(), yTr()
            for g in range(2):
                hs = slice(g * 512, (g + 1) * 512)
                k.stt(yn.t[:, hs], y1.t[:, hs], sm.t[:, 6 + g:7 + g], snB.t[:, hs], ALU.mult, ALU.mult, [y1, sm, snB], [yn])
            for half in range(2):
                for j in range(4):
                    jj = half * 4 + j
                    k.tr(ptr.t[:, j * 128:(j + 1) * 128], yn.t[:, jj * 128:(jj + 1) * 128], identb.t[:], [yn, identb], [ptr])
                k.act(yT.t[:, half * 512:(half + 1) * 512], ptr.t[:], AF.Copy, [ptr], [yT])
            k.dma(io["ynT"].t[:, tok].rearrange("(j p) t -> p j t", p=128), yT.t[:].rearrange("p (j t) -> p j t", j=8),
                  [yT], [io["ynT"]])
        S.emit()


def load_w_bf16(k, dst, src3, nk, ncols, stg, eng="act"):
    for kk in range(nk):
        for c0 in range(0, ncols, 2048):
            w = min(2048, ncols - c0)
            s = stg()
            k.dma(s.t[:, 0:w], src3[:, kk, c0:c0 + w], [], [s])
            k.cp(dst.t[:, kk, c0:c0 + w], s.t[:, 0:w], [s], [dst], eng=eng)


def emit_phase_E(k, io, l, xin):
    S = k.S
    with k.phase():
        stg = k.rot([k.sb([128, 2048], F32, "stg") for _ in range(2)])
        WAO = k.sb([128, 4, D], BF16, "WAO")
        WSO = k.sb([128, 8, D], BF16, "WSO")
        WO = k.sb([128, 8, D], BF16, "WO")
        load_w_bf16(k, WAO, io["w_attn_o"].t[l].rearrange("(k p) n -> p k n", p=128), 4, D, stg)
        load_w_bf16(k, WSO, io["w_ssm_o"].t[l].rearrange("(k p) n -> p k n", p=128), 8, D, stg)
        load_w_bf16(k, WO, io["w_out"].t[l].rearrange("(k p) n -> p k n", p=128), 8, D, stg)
        gmB = k.sb([128, D], F32, "gmB")
        k.dma(gmB.t[:], io["modD"].t[l:l + 1, 2048:3072].partition_broadcast(128), [io["modD"]], [gmB])
        aTr = k.rot([k.sb([128, 4, 512], BF16, "aT") for _ in range(2)])
        yTr = k.rot([k.sb([128, 8, 512], BF16, "yT") for _ in range(2)])
        gTr = k.rot([k.sb([128, 16, 512], BF16, "gT") for _ in range(2)])
        mxr = k.rot([k.sb([128, 8, 512], BF16, "mx") for _ in range(2)])
        m1r = k.rot([k.sb([128, 512], F32, "m1") for _ in range(3)])
        m2r = k.rot([k.sb([128, 512], F32, "m2") for _ in range(3)])
        ysr = k.rot([k.sb([128, 512], F32, "ys") for _ in range(3)])
        xr = k.rot([k.sb([128, D], F32, "x") for _ in range(2)])
        x1r = k.rot([k.sb([128, D], F32, "x1") for _ in range(2)])
        par = k.rot([k.ps() for _ in range(2)])
        psr = k.rot([k.ps() for _ in range(2)])
        por = k.rot([k.ps() for _ in range(3)])
        for tb in range(8):
            ts_ = slice(tb * 512, (tb + 1) * 512)
            aT, yT, gT, mx = aTr(), yTr(), gTr(), mxr()
            k.dma(aT.t[:], io["attT"].t[:, ts_].rearrange("(j p) t -> p j t", p=128), [io["attT"]], [aT])
            k.dma(yT.t[:], io["ynT"].t[:, ts_].rearrange("(j p) t -> p j t", p=128), [io["ynT"]], [yT])
            k.dma(gT.t[:], io["gT"].t[:, ts_].rearrange("(j p) t -> p j t", p=128), [io["gT"]], [gT])
            for f in range(8):
                fs = slice(f * 128, (f + 1) * 128)
                pa, ps = par(), psr()
                for kk in range(4):
                    k.mm(pa.t[:], WAO.t[:, kk, fs], aT.t[:, kk, :], kk == 0, kk == 3, [WAO, aT], [pa])
                for kk in range(8):
                    k.mm(ps.t[:], WSO.t[:, kk, fs], yT.t[:, kk, :], kk == 0, kk == 7, [WSO, yT], [ps])
                m1, m2, ys = m1r(), m2r(), ysr()
                k.tt(m1.t[:], pa.t[:], gT.t[:, f, :], ALU.mult, [pa, gT], [m1])
                k.act(ys.t[:], ps.t[:], AF.Copy, [ps], [ys])
                k.tt(m2.t[:], ys.t[:], gT.t[:, 8 + f, :], ALU.mult, [ys, gT], [m2])
                k.tt(mx.t[:, f, :], m1.t[:], m2.t[:], ALU.add, [m1, m2], [mx])
            for tt in range(4):
                ti = tb * 4 + tt
                tok = slice(ti * 128, (ti + 1) * 128)
                x_, x1 = xr(), x1r()
                k.dma(x_.t[:], xin.t[tok, :], [xin], [x_])
                for n2 in range(2):
                    ns = slice(n2 * 512, (n2 + 1) * 512)
                    po = por()
                    for kk in range(8):
                        k.mm(po.t[:], mx.t[:, kk, tt * 128:(tt + 1) * 128], WO.t[:, kk, ns], kk == 0, kk == 7,
                             [mx, WO], [po])
                    k.tt(x1.t[:, ns], po.t[:], gmB.t[:, ns], ALU.mult, [po, gmB], [x1])
                    k.tt(x1.t[:, ns], x1.t[:, ns], x_.t[:, ns], ALU.add, [x1, x_], [x1])
                k.dma(io["x1"].t[tok, :], x1.t[:], [x1], [io["x1"]])
        S.emit()


def emit_phase_M(k, io, l, xout):
    S = k.S
    gs = io["GS"][l]
    cp = io["colp_sb"][l]
    rp = io["rowp_sb"][l]
    ident = io["ident"]
    NQ = 4
    TQ = L // NQ
    NTQ = TQ // 128
    for Q in range(NQ):
        with k.phase():
            hTq = k.sb([128, 8, TQ], BF16, "hTq", persist=True)
            acc = k.sb([128, NTQ, D], F32, "acc", persist=True)
            GD = k.sb([128, NTQ, 32], F32, "GD", persist=True)
            with k.sub():
                wr = k.sb([128, 8, 32], F32, "wr")
                k.dma(wr.t[:], io["w_router"].t[l].rearrange("(k p) e -> p k e", p=128), [], [wr])
                bdn = k.sb([32, D], F32, "bdn")
                k.dma(bdn.t[:], io["b_dn"].t[l], [], [bdn])
                xt = k.rot([k.sb([128, D], F32, "xt") for _ in range(2)])
                xs = k.rot([k.sb([128, D], F32, "xs") for _ in range(2)])
                sq = k.sb([128, D], F32, "sqj")
                ssb = k.rot([k.sb([128, 4], F32) for _ in range(2)])
                tmpb = k.rot([k.sb([128, 4], F32) for _ in range(2)])
                h32r = k.rot([k.sb([128, 8, 128], F32, "h32") for _ in range(2)])
                lgr = k.rot([k.sb([128, 32], F32, "lg") for _ in range(2)])
                exr = k.rot([k.sb([128, 32], F32, "ex") for _ in range(2)])
                mkr = k.rot([k.sb([128, 32], F32, "mk") for _ in range(2)])
                m8r = k.rot([k.sb([128, 16], F32, "m8") for _ in range(2)])
                gtr = k.rot([k.sb([32, 128], F32, "gdT") for _ in range(2)])
                tp = [k.ps() for _ in range(2)]
                pl = k.rot([k.ps() for _ in range(2)])
                pg = k.rot([k.ps() for _ in range(2)])
                pb = k.rot([k.ps() for _ in range(2)])
                for i in range(NTQ):
                    ti = Q * NTQ + i
                    tok = slice(ti * 128, (ti + 1) * 128)
                    x_, xs_, ss, tmp, h32 = xt(), xs(), ssb(), tmpb(), h32r()
                    k.dma(x_.t[:], io["x1"].t[tok, :], [io["x1"]], [x_])
                    norm_tile(k, x_, xs_, sq, ss, tmp)
                    for half in range(2):
                        p = tp[half]
                        for j in range(4):
                            kk = half * 4 + j
                            k.tr(p.t[:, j * 128:(j + 1) * 128], xs_.t[:, kk * 128:(kk + 1) * 128], ident.t[:],
                                 [xs_, ident], [p])
                        for j in range(4):
                            kk = half * 4 + j
                            k.ts(h32.t[:, kk, :], p.t[:, j * 128:(j + 1) * 128], gs.t[:, 16 + kk:17 + kk],
                                 gs.t[:, 24 + kk:25 + kk], ALU.mult, ALU.add, [p, gs], [h32])
                    k.cp(hTq.t[:, :, i * 128:(i + 1) * 128], h32.t[:], [h32], [hTq], eng="pool")
                    p = pl()
                    for kk in range(8):
                        k.mm(p.t[:, 0:32], h32.t[:, kk, :], wr.t[:, kk, :], kk == 0, kk == 7, [h32, wr], [p])
                    lg, ex, mk, m8 = lgr(), exr(), mkr(), m8r()
                    k.tt(lg.t[:], p.t[:, 0:32], rp.t[:, RP_BR:RP_BR + 32], ALU.add, [p, rp], [lg])
                    S.dve(lambda e, m8=m8, lg=lg: e.max(out=m8.t[:, 0:8], in_=lg.t[:]), [lg.b], [m8.b])
                    k.ts(m8.t[:, 8:9], m8.t[:, 0:1], -1.0, None, ALU.mult, None, [m8], [m8])
                    k.act(ex.t[:], lg.t[:], AF.Exp, [lg, m8], [ex], bias=m8.t[:, 8:9])
                    k.ts(mk.t[:], lg.t[:], m8.t[:, 3:4], None, ALU.is_ge, None, [lg, m8], [mk])
                    k.tt(ex.t[:], ex.t[:], mk.t[:], ALU.mult, [ex, mk], [ex])
                    S.dve(lambda e, m8=m8, ex=ex: e.reduce_sum(out=m8.t[:, 9:10], in_=ex.t[:], axis=AX.X), [ex.b], [m8.b])
                    k.rcp(m8.t[:, 10:11], m8.t[:, 9:10], [m8], [m8])
                    k.ts(GD.t[:, i, :], ex.t[:], m8.t[:, 10:11], None, ALU.mult, None, [ex, m8], [GD])
                    q = pg()
                    k.tr(q.t[0:32, 0:128], GD.t[:, i, :], ident.t[:], [GD, ident], [q])
                    gt = gtr()
                    k.act(gt.t[:], q.t[0:32, 0:128], AF.Copy, [q], [gt])
                    for n2 in range(2):
                        ns = slice(n2 * 512, (n2 + 1) * 512)
                        b_ = pb()
                        k.mm(b_.t[:], gt.t[:], bdn.t[:, ns], True, True, [gt, bdn], [b_])
                        k.act(acc.t[:, i, ns], b_.t[:], AF.Copy, [b_], [acc])
                S.emit()
            with k.sub():
                stg = k.rot([k.sb([128, 2048], F32, "stg") for _ in range(2)])
                WGr = k.rot([k.sb([128, 8, 2 * D], BF16, "WG") for _ in range(2)])
                WDr = k.rot([k.sb([128, 8, D], BF16, "WD") for _ in range(2)])
                aTr = k.rot([k.sb([128, 8, 512], BF16, "actT") for _ in range(1)])
                gqr = k.rot([k.sb([128, 512], F32, "gq") for _ in range(2)])
                sgr = k.rot([k.sb([128, 512], BF16, "sg") for _ in range(2)])
                uqr = k.rot([k.sb([128, 512], BF16, "uq") for _ in range(2)])
                pgr = k.rot([k.ps() for _ in range(2)])
                pur = k.rot([k.ps() for _ in range(2)])
                pdr = k.rot([k.ps() for _ in range(3)])
                for e in range(32):
                    WG, WD = WGr(), WDr()
                    load_w_bf16(k, WG, io["w_gu"].t[l, e].rearrange("(k p) n -> p k n", p=128), 8, 2 * D, stg, eng="act")
                    load_w_bf16(k, WD, io["w_dn"].t[l, e].rearrange("(k p) n -> p k n", p=128), 8, D, stg, eng="act")
                    for tb in range(TQ // 512):
                        hs = slice(tb * 512, (tb + 1) * 512)
                        aT = aTr()
                        for ct in range(8):
                            pg_, pu_ = pgr(), pur()
                            for kk in range(8):
                                k.mm(pg_.t[:], WG.t[:, kk, ct * 128:(ct + 1) * 128], hTq.t[:, kk, hs], kk == 0, kk == 7,
                                     [WG, hTq], [pg_])
                            for kk in range(8):
                                k.mm(pu_.t[:], WG.t[:, kk, D + ct * 128:D + (ct + 1) * 128], hTq.t[:, kk, hs], kk == 0,
                                     kk == 7, [WG, hTq], [pu_])
                            gq, sg, uq = gqr(), sgr(), uqr()
                            bg = cp.t[:, CP_BGU + e * 16 + ct:CP_BGU + e * 16 + ct + 1]
                            bu = cp.t[:, CP_BGU + e * 16 + 8 + ct:CP_BGU + e * 16 + 8 + ct + 1]
                            k.ts(gq.t[:], pg_.t[:], bg, 7.0, ALU.add, ALU.min, [pg_, cp], [gq])
                            k.act(sg.t[:], gq.t[:], AF.Sigmoid, [gq], [sg], scale=1.702)
                            k.ts(uq.t[:], pu_.t[:], bu, 7.0, ALU.add, ALU.min, [pu_, cp], [uq])
                            k.ts(uq.t[:], uq.t[:], -7.0, 1.0, ALU.max, ALU.add, [uq], [uq])
                            k.tt(gq.t[:], gq.t[:], sg.t[:], ALU.mult, [gq, sg], [gq])
                            k.tt(aT.t[:, ct, :], gq.t[:], uq.t[:], ALU.mult, [gq, uq], [aT])
                        for tt in range(4):
                            i = tb * 4 + tt
                            for n2 in range(2):
                                ns = slice(n2 * 512, (n2 + 1) * 512)
                                pd = pdr()
                                for kk in range(8):
                                    k.mm(pd.t[:], aT.t[:, kk, tt * 128:(tt + 1) * 128], WD.t[:, kk, ns], kk == 0, kk == 7,
                                         [aT, WD], [pd])
                                k.stt(acc.t[:, i, ns], pd.t[:], GD.t[:, i, e:e + 1], acc.t[:, i, ns], ALU.mult, ALU.add,
                                      [pd, GD, acc], [acc])
                S.emit()
            with k.sub():
                gfB = k.sb([128, D], F32, "gfB")
                k.dma(gfB.t[:], io["modD"].t[l:l + 1, 5120:6144].partition_broadcast(128), [io["modD"]], [gfB])
                xt = k.rot([k.sb([128, D], F32, "xt") for _ in range(2)])
                x2r = k.rot([k.sb([128, D], F32, "x2") for _ in range(2)])
                for i in range(NTQ):
                    ti = Q * NTQ + i
                    tok = slice(ti * 128, (ti + 1) * 128)
                    x_, x2 = xt(), x2r()
                    k.dma(x_.t[:], io["x1"].t[tok, :], [io["x1"]], [x_])
                    k.tt(x2.t[:], acc.t[:, i, :], gfB.t[:], ALU.mult, [acc, gfB], [x2])
                    k.tt(x2.t[:], x2.t[:], x_.t[:], ALU.add, [x2, x_], [x2], eng="pool")
                    k.dma(xout.t[tok, :], x2.t[:], [x2], [xout])
                S.emit()


CC_SLT = 1920
RP_EOFF = 80
SLOTS = 4096


def emit_phase_MS(k, io, l, xout):
    S = k.S
    gs = io["GS"][l]
    rp = io["rowp_sb"][l]
    ident = io["ident"]
    identb = io["identb"]
    Hs, Ys = io["Hs"], io["Ys"]
    with k.phase():
        DK = k.sb([128, NT, 4], I32, "DK", persist=True)
        GK = k.sb([128, NT, 4], F32, "GK", persist=True)
        with k.sub():
            cst = k.sb([128, 256], F32, "cstM")
            k.dma(cst.t[:, 0:128], io["consts"].t[:, CC_SLT:CC_SLT + 128], [], [cst])
            k.dma(cst.t[:, 128:256], io["consts"].t[:, CC_ONES:CC_ONES + 128], [], [cst])
            SLT = cst.t[:, 0:128]
            ON = cst.t[:, 128:256]
            wr = k.sb([128, 8, 32], F32, "wr")
            k.dma(wr.t[:], io["w_router"].t[l].rearrange("(k p) e -> p k e", p=128), [], [wr])
            GfB = k.sb([128, D], F32, "GfB")
            SfB = k.sb([128, D], F32, "SfB")
            nfB = k.sb([128, D], F32, "nfB")
            k.dma(GfB.t[:], io["modD"].t[l:l + 1, 4096:5120].partition_broadcast(128), [io["modD"]], [GfB])
            k.dma(SfB.t[:], io["modD"].t[l:l + 1, 3072:4096].partition_broadcast(128), [io["modD"]], [SfB])
            k.dma(nfB.t[:], io["norm_ffn"].t[l:l + 1, :].partition_broadcast(128), [], [nfB])
            k.stt(GfB.t[:], GfB.t[:], 1.0, nfB.t[:], ALU.add, ALU.mult, [GfB, nfB], [GfB])
            base = k.sb([128, 32], F32, "base")
            k.ms(base.t[:], 0.0, [base], eng="dve")
            xt = k.rot([k.sb([128, D], F32, "xt") for _ in range(2)])
            xs = k.rot([k.sb([128, D], F32, "xs") for _ in range(2)])
            sq = k.sb([128, D], F32, "sqj")
            ssb = k.rot([k.sb([128, 4], F32) for _ in range(2)])
            tmpb = k.rot([k.sb([128, 4], F32) for _ in range(2)])
            h32r = k.rot([k.sb([128, 8, 128], F32, "h32") for _ in range(2)])
            hrr = k.rot([k.sb([128, D], BF16, "hrow") for _ in range(3)])
            lgr = k.rot([k.sb([128, 32], F32, "lg") for _ in range(2)])
            inr = k.rot([k.sb([128, 32], F32, "ind") for _ in range(2)])
            vlr = k.rot([k.sb([128, 32], F32, "val") for _ in range(2)])
            jkr = k.rot([k.sb([128, 32], F32, "junk") for _ in range(2)])
            m8r = k.rot([k.sb([128, 24], F32, "m8") for _ in range(2)])
            tp = [k.ps() for _ in range(2)]
            pl = k.rot([k.ps() for _ in range(2)])
            pr = k.rot([k.ps() for _ in range(2)])
            for i in range(NT):
                tok = slice(i * 128, (i + 1) * 128)
                x_, xs_, ss, tmp, h32, hrow = xt(), xs(), ssb(), tmpb(), h32r(), hrr()
                k.dma(x_.t[:], io["x1"].t[tok, :], [io["x1"]], [x_])
                norm_tile(k, x_, xs_, sq, ss, tmp)
                for half in range(2):
                    p = tp[half]
                    for j in range(4):
                        kk = half * 4 + j
                        k.tr(p.t[:, j * 128:(j + 1) * 128], xs_.t[:, kk * 128:(kk + 1) * 128], ident.t[:], [xs_, ident], [p])
                    for j in range(4):
                        kk = half * 4 + j
                        k.ts(h32.t[:, kk, :], p.t[:, j * 128:(j + 1) * 128], gs.t[:, 16 + kk:17 + kk],
                             gs.t[:, 24 + kk:25 + kk], ALU.mult, ALU.add, [p, gs], [h32])
                k.tt(sq.t[:], xs_.t[:], GfB.t[:], ALU.mult, [xs_, GfB], [sq])
                k.tt(hrow.t[:], sq.t[:], SfB.t[:], ALU.add, [sq, SfB], [hrow])
                p = pl()
                for kk in range(8):
                    k.mm(p.t[:, 0:32], h32.t[:, kk, :], wr.t[:, kk, :], kk == 0, kk == 7, [h32, wr], [p])
                lg, ind, val, junk, m8 = lgr(), inr(), vlr(), jkr(), m8r()
                k.tt(lg.t[:], p.t[:, 0:32], rp.t[:, RP_BR:RP_BR + 32], ALU.add, [p, rp], [lg])
                S.dve(lambda e, m8=m8, lg=lg: e.max(out=m8.t[:, 0:8], in_=lg.t[:]), [lg.b], [m8.b])
                k.ts(m8.t[:, 8:9], m8.t[:, 0:1], -1.0, None, ALU.mult, None, [m8], [m8])
                k.act(m8.t[:, 12:16], m8.t[:, 0:4], AF.Exp, [m8], [m8], bias=m8.t[:, 8:9])
                S.dve(lambda e, m8=m8: e.reduce_sum(out=m8.t[:, 9:10], in_=m8.t[:, 12:16], axis=AX.X), [m8.b], [m8.b])
                k.rcp(m8.t[:, 10:11], m8.t[:, 9:10], [m8], [m8])
                k.ts(GK.t[:, i, :], m8.t[:, 12:16], m8.t[:, 10:11], None, ALU.mult, None, [m8], [GK])
                k.ts(ind.t[:], lg.t[:], m8.t[:, 3:4], None, ALU.is_ge, None, [lg, m8], [ind])
                q = pr()
                k.mm(q.t[:, 0:32], SLT, ind.t[:], True, True, [cst, ind], [q])
                k.mm(q.t[:, 32:64], ON, ind.t[:], True, True, [cst, ind], [q])
                k.tt(val.t[:], q.t[:, 0:32], base.t[:], ALU.add, [q, base], [val])
                k.tt(val.t[:], val.t[:], rp.t[:, RP_EOFF:RP_EOFF + 32], ALU.add, [val, rp], [val])
                k.tt(base.t[:], base.t[:], q.t[:, 32:64], ALU.add, [base, q], [base])
                for kq in range(4):
                    k.ts(junk.t[:], lg.t[:], m8.t[:, kq:kq + 1], None, ALU.is_equal, None, [lg, m8], [junk])
                    k.tt(junk.t[:], junk.t[:], val.t[:], ALU.mult, [junk, val], [junk])
                    S.dve(lambda e, m8=m8, junk=junk, kq=kq: e.reduce_sum(out=m8.t[:, 16 + kq:17 + kq], in_=junk.t[:],
                                                                          axis=AX.X), [junk.b], [m8.b])
                k.cp(DK.t[:, i, :], m8.t[:, 16:20], [m8], [DK])
                for kq in range(4):
                    S.dma(lambda e, i=i, kq=kq, hrow=hrow: e.indirect_dma_start(
                        out=Hs.t, out_offset=bass.IndirectOffsetOnAxis(ap=DK.t[:, i, kq:kq + 1], axis=0),
                        in_=hrow.t[:], in_offset=None), [hrow.b, DK.b], [], q="pool")
            cnti = k.sb([128, 32], I32, "cnti")
            k.cp(cnti.t[:], base.t[:], [base], [cnti])
            k.dma(io["cntD"].t, cnti.t[0:1, :], [cnti], [io["cntD"]])
            S.emit()
        with k.sub():
            cnt_sb = k.sb([1, 32], I32, "cnt_sb")
            k.dma(cnt_sb.t[:], io["cntD"].t, [io["cntD"]], [cnt_sb])
            onesb = k.sb([1, 128], BF16, "onesb")
            k.ms(onesb.t[:], 1.0, [onesb], eng="dve")
            stg = k.rot([k.sb([128, 2048], F32, "stg") for _ in range(3)])
            WGr = k.rot([k.sb([128, 8, 2 * D], BF16, "WG") for _ in range(2)])
            WDr = k.rot([k.sb([128, 8, D], BF16, "WD") for _ in range(2)])
            bgr = k.rot([k.sb([1, 3 * D], BF16, "bgr") for _ in range(2)])
            hbr = k.rot([k.sb([128, D], BF16, "hb") for _ in range(3)])
            hTr = k.rot([k.sb([128, 8, 128], BF16, "hTe") for _ in range(2)])
            aThr = k.rot([k.sb([128, 4, 128], BF16, "actT") for _ in range(4)])
            gqr = k.rot([k.sb([128, 512], F32, "gq") for _ in range(2)])
            sgr = k.rot([k.sb([128, 512], BF16, "sg") for _ in range(2)])
            uqr = k.rot([k.sb([128, 512], F32, "uq") for _ in range(2)])
            ybr = k.rot([k.sb([128, D], BF16, "yb") for _ in range(2)])
            acthr = k.rot([k.sb([128, 512], BF16, "acth") for _ in range(4)])
            ptr = [k.ps([128, 512], BF16, "ptr") for _ in range(2)]
            pgr = k.rot([k.ps() for _ in range(2)])
            pur = k.rot([k.ps() for _ in range(2)])
            pdr = k.rot([k.ps() for _ in range(2)])
            def wchunks(e):
                WG, WD = Wbuf[e % 2]
                wg3 = io["w_gu"].t[l, e].rearrange("(k p) n -> p k n", p=128)
                wd3 = io["w_dn"].t[l, e].rearrange("(k p) n -> p k n", p=128)
                out = []
                for kk in range(8):
                    out.append((WG, WG.t[:, kk, :], wg3[:, kk, :], 2 * D))
                    out.append((WD, WD.t[:, kk, :], wd3[:, kk, :], D))
                return out

            def chunk_dma(ch):
                dstT, dst_ap, src_ap, w = ch
                s_ = stg()
                k.dma(s_.t[:, 0:w], src_ap, [], [s_])
                return (dstT, dst_ap, s_, w)

            def chunk_cast(c):
                dstT, dst_ap, s_, w = c
                k.cp(dst_ap, s_.t[:, 0:w], [s_], [dstT], eng="act")

            def load_chunk(ch):
                chunk_cast(chunk_dma(ch))

            def grp_of(e, j, gid0):
                inner = (gid0 + j, j * 128)
                if j < 4:
                    return (inner,)
                lo = 4 if j < 8 else (8 if j < 16 else 16)
                return ((gid0 + 100 + lo, lo * 128), inner)

            def load_block(e, j, gid0):
                S.group = grp_of(e, j, gid0)
                hb = hbr()
                r0 = e * SLOTS + j * 128
                k.dma(hb.t[:], Hs.t[r0:r0 + 128, :], [], [hb])
                S.group = None
                return hb

            Wbuf = [(WGr(), WDr()), (WGr(), WDr())]
            S.group = None
            for ch in wchunks(0):
                load_chunk(ch)
            gid = 0
            for e in range(32):
                S.group = None
                WG, WD = Wbuf[e % 2]
                br = bgr()
                k.dma(br.t[0:1, 0:2 * D], io["b_gu"].t[l, e:e + 1, :], [], [br], q="pool")
                k.dma(br.t[0:1, 2 * D:3 * D], io["b_dn"].t[l, e:e + 1, :], [], [br], q="pool")
                S.regload(cnt_sb.t[0:1, e:e + 1], [cnt_sb.b])
                nxt = wchunks(e + 1) if e + 1 < 32 else []
                gid0 = gid + 1
                gid += 1000
                hb_next = load_block(e, 0, gid0)
                for j in range(SLOTS // 128):
                    hb = hb_next
                    if j + 1 < SLOTS // 128:
                        hb_next = load_block(e, j + 1, gid0)
                    pend = []
                    if j < 6:
                        S.group = None
                        for ch in nxt[3 * j:min(3 * j + 3, 16)]:
                            pend.append(chunk_dma(ch))
                    S.group = grp_of(e, j, gid0)
                    r0 = e * SLOTS + j * 128
                    hT, yb = hTr(), ybr()
                    aTh = [aThr(), aThr()]
                    for half in range(2):
                        for c4 in range(4):
                            kk = half * 4 + c4
                            k.tr(ptr[half].t[:, c4 * 128:(c4 + 1) * 128], hb.t[:, kk * 128:(kk + 1) * 128], identb.t[:],
                                 [hb, identb], [ptr[half]])
                        k.act(hT.t[:, half * 4:(half + 1) * 4, :], ptr[half].t[:].rearrange("p (a s) -> p a s", a=4),
                              AF.Copy, [ptr[half]], [hT])
                    pgu = [pgr(), pgr(), pur(), pur()]
                    for n4 in (0, 2, 1, 3):
                        cs = slice(n4 * 512, (n4 + 1) * 512)
                        k.mm(pgu[n4].t[:], onesb.t[0:1, :], br.t[0:1, cs], True, False, [br, onesb], [pgu[n4]])
                        for kk in range(8):
                            k.mm(pgu[n4].t[:], hT.t[:, kk, :], WG.t[:, kk, cs], False, kk == 7, [hT, WG], [pgu[n4]])
                    acth = [acthr(), acthr()]
                    for hh in range(2):
                        gq, sg, uq = gqr(), sgr(), uqr()
                        k.ts(gq.t[:], pgu[hh].t[:], 7.0, None, ALU.min, None, [pgu[hh]], [gq])
                        k.act(sg.t[:], gq.t[:], AF.Sigmoid, [gq], [sg], scale=1.702)
                        k.ts(uq.t[:], pgu[2 + hh].t[:], 7.0, -7.0, ALU.min, ALU.max, [pgu[2 + hh]], [uq])
                        k.tt(gq.t[:], gq.t[:], sg.t[:], ALU.mult, [gq, sg], [gq])
                        k.stt(acth[hh].t[:], uq.t[:], 1.0, gq.t[:], ALU.add, ALU.mult, [uq, gq], [acth[hh]])
                    for half in range(2):
                        act = acth[half]
                        for c4 in range(4):
                            kk = half * 4 + c4
                            k.tr(ptr[half].t[:, c4 * 128:(c4 + 1) * 128], act.t[:, c4 * 128:(c4 + 1) * 128], identb.t[:],
                                 [act, identb], [ptr[half]])
                        k.act(aTh[half].t[:], ptr[half].t[:].rearrange("p (a s) -> p a s", a=4),
                              AF.Copy, [ptr[half]], [aTh[half]])
                    for n2 in range(2):
                        ns = slice(n2 * 512, (n2 + 1) * 512)
                        pd = pdr()
                        k.mm(pd.t[:], onesb.t[0:1, :], br.t[0:1, 2 * D + n2 * 512:2 * D + (n2 + 1) * 512], True, False,
                             [br, onesb], [pd])
                        for kk in range(8):
                            k.mm(pd.t[:], aTh[kk // 4].t[:, kk % 4, :], WD.t[:, kk, ns], False, kk == 7, [aTh[kk // 4], WD], [pd])
                        k.act(yb.t[:, ns], pd.t[:], AF.Copy, [pd], [yb])
                    k.dma(Ys.t[r0:r0 + 128, :], yb.t[:], [yb], [])
                    S.group = None
                    for c in pend:
                        chunk_cast(c)
                S.group = None
            S.emit()
        with k.sub():
            gfB = k.sb([128, D], F32, "gfB")
            k.dma(gfB.t[:], io["modD"].t[l:l + 1, 5120:6144].partition_broadcast(128), [io["modD"]], [gfB])
            xt = k.rot([k.sb([128, D], F32, "xt") for _ in range(2)])
            x2r = k.rot([k.sb([128, D], F32, "x2") for _ in range(2)])
            acr = k.rot([k.sb([128, D], F32, "acc") for _ in range(2)])
            rwr = k.rot([k.sb([128, D], BF16, "rw") for _ in range(8)])
            for i in range(NT):
                tok = slice(i * 128, (i + 1) * 128)
                x_, x2, acc = xt(), x2r(), acr()
                k.dma(x_.t[:], io["x1"].t[tok, :], [io["x1"]], [x_])
                for kq in range(4):
                    rw = rwr()
                    S.dma(lambda e, i=i, kq=kq, rw=rw: e.indirect_dma_start(
                        out=rw.t[:], out_offset=None, in_=Ys.t,
                        in_offset=bass.IndirectOffsetOnAxis(ap=DK.t[:, i, kq:kq + 1], axis=0)), [DK.b], [rw.b],
                        q="pool")
                    if kq == 0:
                        k.ts(acc.t[:], rw.t[:], GK.t[:, i, 0:1], None, ALU.mult, None, [rw, GK], [acc])
                    else:
                        k.stt(acc.t[:], rw.t[:], GK.t[:, i, kq:kq + 1], acc.t[:], ALU.mult, ALU.add, [rw, GK, acc], [acc])
                k.tt(x2.t[:], acc.t[:], gfB.t[:], ALU.mult, [acc, gfB], [x2])
                k.tt(x2.t[:], x2.t[:], x_.t[:], ALU.add, [x2, x_], [x2])
                k.dma(xout.t[tok, :], x2.t[:], [x2], [xout])
            S.emit()


def declare_io(k, dbg=(), ne=32):
    nc = k.nc
    io = {}

    def ext(name, shape, dt=F32):
        io[name] = k.dram(name, shape, dt, kind="ExternalInput")

    ext("x", [L, D]); ext("ccols", [128, 8]); ext("rel_bias", [32, 8])
    ext("w_ada", [2, D, 6 * D]); ext("b_ada", [2, 6 * D]); ext("w_in", [2, D, DIN])
    ext("w_kv_up", [2, 256, 1024]); ext("w_attn_o", [2, 512, D]); ext("w_ssm_o", [2, D, D]); ext("w_out", [2, D, D])
    ext("w_router", [2, D, 32]); ext("w_gu", [2, ne, D, 2 * D]); ext("w_dn", [2, ne, D, D]); ext("b_dn", [2, 32, D])
    ext("colp", [2, 128, NCOLP]); ext("rowp", [2, 128, NROWP]); ext("norm_ffn", [2, D]); ext("ssm_norm", [2, D])
    ext("consts", [128, 2048]); ext("b_gu", [2, 32, 2 * D])

    def scr(name, shape, dt):
        kind = "ExternalOutput" if name in dbg else "Internal"
        io[name] = k.dram(name, shape, dt, kind=kind)

    scr("modD", [2, 6 * D], F32)
    scr("qT", [512, L], BF16); scr("kT", [512, L], BF16); scr("v", [L, 512], BF16); scr("qiT", [512, L], BF16)
    scr("kiT", [64, L], BF16); scr("wi", [L, 8], F32); scr("z", [L, D], BF16); scr("xs", [L, D], BF16)
    scr("Btok", [L, 256], BF16); scr("BT", [256, L], BF16); scr("CT", [256, L], BF16); scr("dt", [L, 32], F32)
    scr("gT", [2048, L], BF16); scr("attT", [512, L], BF16); scr("ynT", [D, L], BF16)
    scr("Gd", [8, 384], F32); scr("Hs", [32 * SLOTS, D], BF16); scr("Ys", [32 * SLOTS, D], BF16); scr("cntD", [1, 32], I32); scr("x1", [L, D], F32); scr("xmid", [L, D], F32)
    io["out"] = k.dram("out", [L, D], F32, kind="ExternalOutput")
    io["ident"] = k.psb([128, 128], F32, "ident")
    io["identb"] = k.psb([128, 128], BF16, "identb")
    io["bd64"] = k.psb([128, 128], F32, "bd64")
    io["on256"] = k.psb([128, 128], F32, "on256")
    io["epsc"] = k.psb([128, 1], F32, "epsc")
    io["onec"] = k.psb([128, 1], F32, "onec")
    io["colp_sb"] = [k.psb([128, NCOLP], F32, f"colp{l}") for l in range(2)]
    io["rowp_sb"] = [k.psb([128, NROWP], F32, f"rowp{l}") for l in range(2)]
    io["GS"] = [k.psb([128, 32], F32, f"GS{l}") for l in range(2)]
    io["EBd"] = k.psb([128, 8, 128], BF16, "EBd")
    io["EBo"] = k.psb([128, 8, 128], BF16, "EBo")
    with k.phase():
        cst = k.sb([128, 1024], F32)
        k.dma(cst.t[:], io["consts"].t[:, 0:1024], [], [cst])
        k.cp(io["ident"].t[:], cst.t[:, 0:128], [cst], [io["ident"]])
        k.cp(io["identb"].t[:], cst.t[:, 0:128], [cst], [io["identb"]])
        k.cp(io["bd64"].t[:], cst.t[:, 128:256], [cst], [io["bd64"]])
        k.cp(io["on256"].t[:], cst.t[:, 256:384], [cst], [io["on256"]])
        k.cp(io["epsc"].t[:], cst.t[:, 384:385], [cst], [io["epsc"]])
        k.cp(io["onec"].t[:], cst.t[:, 385:386], [cst], [io["onec"]])
        for l in range(2):
            k.dma(io["colp_sb"][l].t[:], io["colp"].t[l], [], [io["colp_sb"][l]])
            k.dma(io["rowp_sb"][l].t[:], io["rowp"].t[l], [], [io["rowp_sb"][l]])
        k.S.emit()
    return io


def host_consts():
    c = np.zeros((128, 2048), np.float32)
    c[:, 512:640] = np.eye(128, dtype=np.float32)[::-1]
    qq = np.arange(128)[:, None]; ssx = np.arange(128)[None, :]
    c[:, 640:768] = np.where(ssx <= qq, 0.0, NEG)
    u = np.arange(384); d = u - 127
    n = np.maximum(d, 0); nf_ = np.maximum(n, 1).astype(np.float32)
    large = 16 + (np.log(nf_ / 16) / np.log(128 / 16) * 16).astype(np.int32)
    large = np.minimum(large, 31)
    bucket = np.where(n < 16, n, large)
    ohm = np.zeros((32, 384), np.float32)
    ohm[bucket, u] += 1.0
    ohm[31, :] -= 1.0
    c[0:32, 768:1152] = ohm
    c[0:8, 1152:1536] = (d >= 0).astype(np.float32)[None, :]
    kq = np.arange(128)[:, None]; iq = np.arange(128)[None, :]
    c[:, 1536:1664] = (kq <= iq).astype(np.float32)
    c[:, 1664:1792] = (kq > iq).astype(np.float32)
    c[:, 1792:1920] = 1.0
    c[:, 1920:2048] = (kq < iq).astype(np.float32)
    c[:, 0:128] = np.eye(128, dtype=np.float32)
    bd = np.zeros((128, 128), np.float32)
    bd[0:64, 0:64] = 1.0 / 64
    bd[64:128, 64:128] = 1.0 / 64
    c[:, 128:256] = bd
    c[:, 256:384] = 1.0 / 256
    c[:, 384] = EPS
    c[:, 385] = 1.0
    return c


def host_inputs(inputs, b, ne=32):
    f = lambda a: np.ascontiguousarray(a, dtype=np.float32)
    colp = np.zeros((2, 128, NCOLP), np.float32)
    rowp = np.zeros((2, 128, NROWP), np.float32)
    for l in range(2):
        colp[l, :, CP_NM:CP_NM + 8] = inputs["norm_mix"][l].reshape(8, 128).T
        colp[l, :, CP_NF:CP_NF + 8] = inputs["norm_ffn"][l].reshape(8, 128).T
        colp[l, :, CP_KVN:CP_KVN + 2] = inputs["kv_norm"][l].reshape(2, 128).T
        colp[l, :, CP_QN] = np.tile(inputs["q_norm"][l], 2)
        colp[l, :, CP_KN] = np.tile(inputs["k_norm"][l], 2)
        colp[l, :, CP_LNW] = np.tile(inputs["idx_k_ln_w"][l], 2)
        colp[l, :, CP_LNB] = np.tile(inputs["idx_k_ln_b"][l], 2)
        colp[l, :, CP_CONVW:CP_CONVW + 48] = inputs["conv_w"][l].T.reshape(12, 128, 4).transpose(1, 0, 2).reshape(128, 48)
        colp[l, :, CP_CONVB:CP_CONVB + 12] = inputs["conv_b"][l].reshape(12, 128).T
        colp[l, :, CP_BGU:CP_BGU + 512] = inputs["b_gu"][l].reshape(32, 16, 128).transpose(2, 0, 1).reshape(128, 512)
        rowp[l, :, RP_DTB:RP_DTB + 16] = inputs["dt_bias"][l][None, :]
        rowp[l, :, RP_ALOG:RP_ALOG + 16] = inputs["a_log"][l][None, :]
        rowp[l, :, RP_DSK:RP_DSK + 16] = inputs["d_skip"][l][None, :]
        rowp[l, :, RP_BR:RP_BR + 32] = inputs["b_router"][l][None, :]
        rowp[l, :, RP_EOFF:RP_EOFF + 32] = (np.arange(32) * SLOTS).astype(np.float32)[None, :]
    d = dict(
        x=f(inputs["x"][b]), ccols=f(inputs["c"][b].reshape(8, 128).T), rel_bias=f(inputs["rel_bias"]),
        w_ada=f(inputs["w_ada"]), b_ada=f(inputs["b_ada"]), w_in=f(inputs["w_in"]), w_kv_up=f(inputs["w_kv_up"]),
        w_attn_o=f(inputs["w_attn_o"]), w_ssm_o=f(inputs["w_ssm_o"]), w_out=f(inputs["w_out"]),
        w_router=f(inputs["w_router"]), w_gu=f(inputs["w_gu"][:, :ne]), w_dn=f(inputs["w_dn"][:, :ne]), b_dn=f(inputs["b_dn"]), b_gu=f(inputs["b_gu"]),
        colp=colp, rowp=rowp, norm_ffn=f(inputs["norm_ffn"]), ssm_norm=f(inputs["ssm_norm"]), consts=host_consts(),
    )
    return d


def build_program(ne=32, dbg=(), layers=2, upto="M", sparse=True):
    nc = bass.Bass("TRN2", target_bir_lowering=False)
    k = K(nc)
    io = declare_io(k, dbg, ne)
    emit_phase_A(k, io)
    emit_phase_C0(k, io)
    xin = io["x"]
    for l in range(layers):
        emit_phase_B(k, io, l, xin)
        emit_phase_C(k, io, l)
        emit_phase_D(k, io, l)
        emit_phase_E(k, io, l, xin)
        if upto == "E":
            break
        xout = io["out"] if l == layers - 1 else io["xmid"]
        if sparse:
            emit_phase_MS(k, io, l, xout)
        else:
            emit_phase_M(k, io, l, xout)
        xin = xout
    return nc, k


_PROG = {}


def kernel(**inputs):
    inputs = {n: np.asarray(v) for n, v in inputs.items()}
    if "nc" not in _PROG:
        _PROG["nc"] = build_program()[0]
    nc = _PROG["nc"]
    B = inputs["x"].shape[0]
    shared = host_inputs(inputs, 0)
    in_maps = []
    for b in range(B):
        d = dict(shared)
        d["x"] = np.ascontiguousarray(inputs["x"][b], dtype=np.float32)
        d["ccols"] = np.ascontiguousarray(inputs["c"][b].reshape(8, 128).T, dtype=np.float32)
        in_maps.append(d)
    res = run_bass_kernel_spmd(nc, in_maps, core_ids=list(range(B)))
    return np.stack([np.asarray(r["out"], dtype=np.float32) for r in res.results], axis=0)
```
